# Optimizing a Trainium2 kernel written in Bass

```python
import math
import jax, jax.numpy as jnp
from jax import lax
import numpy as np

D_MODEL = 1024
BATCH = 8
SEQ = 4096
DEPTH = 1

CONV_C = 1024
CONV_W = 31
FOX_HEADS = 16
FOX_HD = 64
FOX_W = FOX_HEADS * FOX_HD
Q_BLOCK = 128
IN_SPLITS = (CONV_C, CONV_C, FOX_W, FOX_W, FOX_W, FOX_HEADS, D_MODEL, D_MODEL)
IN_COLS = sum(IN_SPLITS)
PEER_HEADS = 8
PEER_NKEYS = 128
PEER_EXPERTS = PEER_NKEYS * PEER_NKEYS
PEER_DK = 256
PEER_TOPK = 16
PEER_CHUNK = 128
EPS = 1e-6
NEG = -1e30

kernel_name = "hybrid_conv_fox_peer_block"


def rmsnorm(x, g):
    xf = x.astype(jnp.float32)
    y = xf * lax.rsqrt(jnp.mean(xf * xf, axis=-1, keepdims=True) + EPS)
    return (y * g.astype(jnp.float32)).astype(x.dtype)


def layernorm(x, g, b):
    xf = x.astype(jnp.float32)
    mu = jnp.mean(xf, axis=-1, keepdims=True)
    var = jnp.mean(jnp.square(xf - mu), axis=-1, keepdims=True)
    y = (xf - mu) * lax.rsqrt(var + EPS)
    return (y * g.astype(jnp.float32) + b.astype(jnp.float32)).astype(x.dtype)


def conformer_conv(z_val, z_gate, conv_w, conv_b, ln_g, ln_b, w_conv_out):
    z = z_val * jax.nn.sigmoid(z_gate)
    z = lax.conv_general_dilated(
        z, conv_w[:, None, :].astype(z.dtype), window_strides=(1,),
        padding=[(CONV_W - 1, 0)], dimension_numbers=("NWC", "WIO", "NWC"),
        feature_group_count=CONV_C) + conv_b
    z = layernorm(z, ln_g, ln_b)
    z = z * jax.nn.sigmoid(z)
    return z @ w_conv_out


def forgetting_attention(q, k, v, c):
    B, H, S, hd = q.shape
    nb = S // Q_BLOCK
    scale = 1.0 / math.sqrt(hd)
    qb = q.reshape(B, H, nb, Q_BLOCK, hd).transpose(2, 0, 1, 3, 4)
    cb = c.reshape(B, H, nb, Q_BLOCK).transpose(2, 0, 1, 3)
    kf = k.astype(jnp.float32)
    vf = v.astype(jnp.float32)
    kpos = jnp.arange(S)

    def one_block(args):
        qi, ci, i = args
        s = jnp.einsum("bhqd,bhkd->bhqk", qi.astype(jnp.float32), kf) * scale
        s = s + ci[..., :, None] - c[:, :, None, :]
        qpos = i * Q_BLOCK + jnp.arange(Q_BLOCK)
        mask = kpos[None, :] <= qpos[:, None]
        p = jax.nn.softmax(jnp.where(mask, s, NEG), axis=-1)
        return jnp.einsum("bhqk,bhkd->bhqd", p, vf)

    out = lax.map(one_block, (qb, cb, jnp.arange(nb)))
    return out.transpose(1, 2, 0, 3, 4).reshape(B, H, S, hd).astype(q.dtype)


def peer_ffn(h, peer_wq, peer_k1, peer_k2, peer_u, peer_v):
    B, S, D = h.shape
    T = B * S
    ht = h.reshape(T, D)
    q = (ht @ peer_wq).reshape(T, PEER_HEADS, PEER_DK).astype(jnp.float32)
    half = PEER_DK // 2
    s1 = jnp.einsum("thd,hnd->thn", q[..., :half], peer_k1.astype(jnp.float32))
    s2 = jnp.einsum("thd,hnd->thn", q[..., half:], peer_k2.astype(jnp.float32))
    v1, i1 = lax.top_k(s1, PEER_TOPK)
    v2, i2 = lax.top_k(s2, PEER_TOPK)
    cand = (v1[..., :, None] + v2[..., None, :]).reshape(T, PEER_HEADS, PEER_TOPK * PEER_TOPK)
    sc, pos = lax.top_k(cand, PEER_TOPK)
    e1 = jnp.take_along_axis(i1, pos // PEER_TOPK, axis=-1)
    e2 = jnp.take_along_axis(i2, pos % PEER_TOPK, axis=-1)
    experts = (e1 * PEER_NKEYS + e2).reshape(T, PEER_HEADS * PEER_TOPK)
    gates = jax.nn.softmax(sc, axis=-1).reshape(T, PEER_HEADS * PEER_TOPK)
    nc = T // PEER_CHUNK

    def chunk(args):
        hc, ec, gc = args
        u = peer_u[ec]
        a = jax.nn.gelu(jnp.einsum("ckd,cd->ck", u, hc).astype(jnp.float32)) * gc
        vv = peer_v[ec]
        return jnp.einsum("ck,ckd->cd", a.astype(vv.dtype), vv)

    out = lax.map(chunk, (ht.reshape(nc, PEER_CHUNK, D),
                          experts.reshape(nc, PEER_CHUNK, -1),
                          gates.reshape(nc, PEER_CHUNK, -1)))
    return out.reshape(B, S, D).astype(h.dtype)


def setup_inputs(seed: int = 0) -> dict:
    key = jax.random.key(seed)
    ks = jax.random.split(key, 20)
    f32 = jnp.float32
    D = D_MODEL
    nrm = lambda k, shape, s: jax.random.normal(k, shape, f32) * s
    return {
        "x": nrm(ks[0], (BATCH, SEQ, D), 1.0),
        "norm1_g": 1.0 + nrm(ks[1], (D,), 0.02),
        "w_in": nrm(ks[2], (D, IN_COLS), D ** -0.5),
        "conv_w": nrm(ks[3], (CONV_W, CONV_C), CONV_W ** -0.5),
        "conv_b": nrm(ks[4], (CONV_C,), 0.02),
        "conv_ln_g": 1.0 + nrm(ks[5], (CONV_C,), 0.02),
        "conv_ln_b": nrm(ks[6], (CONV_C,), 0.02),
        "w_conv_out": nrm(ks[7], (CONV_C, D), CONV_C ** -0.5),
        "fox_bf": 1.0 + 4.0 * jax.random.uniform(ks[8], (FOX_HEADS,), f32),
        "w_fox_out": nrm(ks[9], (FOX_W, D), FOX_W ** -0.5),
        "w_out": nrm(ks[10], (D, D), D ** -0.5),
        "norm2_g": 1.0 + nrm(ks[11], (D,), 0.02),
        "peer_wq": nrm(ks[12], (D, PEER_HEADS * PEER_DK), D ** -0.5),
        "peer_k1": nrm(ks[13], (PEER_HEADS, PEER_NKEYS, PEER_DK // 2), (PEER_DK // 2) ** -0.5),
        "peer_k2": nrm(ks[14], (PEER_HEADS, PEER_NKEYS, PEER_DK // 2), (PEER_DK // 2) ** -0.5),
        "peer_u": nrm(ks[15], (PEER_EXPERTS, D), D ** -0.5),
        "peer_v": nrm(ks[16], (PEER_EXPERTS, D), PEER_HEADS ** -0.5),
        "normf_g": 1.0 + nrm(ks[17], (D,), 0.02),
    }


def reference(x, norm1_g, w_in, conv_w, conv_b, conv_ln_g, conv_ln_b, w_conv_out,
              fox_bf, w_fox_out, w_out, norm2_g, peer_wq, peer_k1, peer_k2,
              peer_u, peer_v, normf_g):
    B, S, D = x.shape
    idx = np.cumsum(IN_SPLITS)[:-1].tolist()
    for _ in range(DEPTH):
        h = rmsnorm(x, norm1_g)
        z = h @ w_in
        a_val, a_gate, q, k, v, f_logit, g_a, g_b = jnp.split(z, idx, axis=-1)
        ya = conformer_conv(a_val, a_gate, conv_w, conv_b, conv_ln_g, conv_ln_b, w_conv_out)
        heads = lambda t: t.reshape(B, S, FOX_HEADS, FOX_HD).transpose(0, 2, 1, 3)
        log_f = jax.nn.log_sigmoid(f_logit.astype(jnp.float32) + fox_bf.astype(jnp.float32))
        c = jnp.cumsum(log_f, axis=1).transpose(0, 2, 1)
        ob = forgetting_attention(heads(q), heads(k), heads(v), c)
        yb = ob.transpose(0, 2, 1, 3).reshape(B, S, FOX_W) @ w_fox_out
        mix = (jax.nn.sigmoid(g_a) * ya + jax.nn.sigmoid(g_b) * yb) @ w_out
        x = x + mix
        x = x + peer_ffn(rmsnorm(x, norm2_g), peer_wq, peer_k1, peer_k2, peer_u, peer_v)
    return rmsnorm(x, normf_g)
```

```python
import os
from contextlib import ExitStack
import numpy as np
import concourse.bass as bass
import concourse.mybir as mybir
from concourse.bass_utils import run_bass_kernel_spmd

F32 = mybir.dt.float32
BF16 = mybir.dt.bfloat16
AF = mybir.ActivationFunctionType
ALU = mybir.AluOpType

S = 4096
D = 1024
NT = S // 128
EPS = 1e-6
PHASES = int(os.environ.get("MK_PHASES", "99"))


class Buf:
    __slots__ = ("lw", "rd")

    def __init__(self):
        self.lw = None
        self.rd = {}


class Arena:
    def __init__(self, ap, nelem):
        self.ap = ap
        self.n = nelem
        self.off = 0

    def alloc(self, shape, dtype=F32):
        size = 4 if dtype == F32 else 2
        nb = size * int(np.prod(shape[1:]))
        nel = (nb + 63) // 64 * 32
        assert self.off + nel <= self.n, ("SBUF arena overflow", self.off, nel, self.n)
        v = self.ap[:, self.off:self.off + nb // 2]
        self.off += nel
        if dtype == F32:
            v = v.bitcast(F32)
        if len(shape) == 3:
            v = v.rearrange("p (a b) -> p a b", a=shape[1])
        elif len(shape) == 4:
            v = v.rearrange("p (a b c) -> p a b c", a=shape[1], b=shape[2])
        return v

    def mark(self):
        return self.off

    def release(self, m):
        self.off = m


class Prog:
    def __init__(self, nc, es, ndma=40):
        self.nc = nc
        self.names = ["sp", "act", "dve", "pool", "pe"]
        self.sem = {k: es.enter_context(nc.semaphore("S_" + k)) for k in self.names}
        self.cnt = {k: 0 for k in self.names}
        self.stream = {k: [] for k in self.names}
        self.seen = {k: {} for k in self.names}
        self.dsem = [es.enter_context(nc.semaphore("D%d" % i)) for i in range(ndma)]
        self.dcnt = [0] * ndma
        self.dnext = 0

    def op(self, e, fn, r=(), w=(), dma=False):
        deps = []
        for b in r:
            if b.lw is not None:
                deps.append(b.lw)
        for b in w:
            if b.lw is not None:
                deps.append(b.lw)
            deps.extend(b.rd.values())
        waits = []
        seen = self.seen[e]
        for (s, v, se) in deps:
            if se == "pe" and e == "pe":
                continue
            if se == e and self.cnt[e] + 1 - v >= 4:
                continue
            key = id(s)
            if seen.get(key, 0) >= v:
                continue
            seen[key] = v
            waits.append((s, v))
        if dma:
            i = self.dnext
            self.dnext = (i + 1) % len(self.dsem)
            s = self.dsem[i]
            pv = self.dcnt[i]
            if pv > 0 and seen.get(id(s), 0) < pv:
                seen[id(s)] = pv
                waits.append((s, pv))
            self.dcnt[i] += 16
            tok = (s, self.dcnt[i], "dma")
            inc = (s, 16)
        else:
            self.cnt[e] += 1
            tok = (self.sem[e], self.cnt[e], e)
            inc = (self.sem[e], 1)
        self.stream[e].append((waits, fn, inc))
        for b in r:
            old = b.rd.get(id(tok[0]))
            if old is None or old[1] < tok[1]:
                b.rd[id(tok[0])] = tok
        for b in w:
            b.lw = tok
            b.rd = {}
        return tok

    def barrier(self):
        targets = [(self.sem[k], self.cnt[k]) for k in self.names if self.cnt[k] > 0]
        targets += [(self.dsem[i], self.dcnt[i]) for i in range(len(self.dsem)) if self.dcnt[i] > 0]
        for e in self.names:
            seen = self.seen[e]
            ws = []
            for s, v in targets:
                if seen.get(id(s), 0) < v:
                    seen[id(s)] = v
                    ws.append((s, v))
            if ws:
                self.stream[e].append((ws, None, None))

    def dma(self, e, out, in_, r=(), w=()):
        return self.op(e, lambda g: g.dma_start(out=out, in_=in_), r=r, w=w, dma=True)

    def mm(self, out, lhsT, rhs, start, stop, r=(), w=()):
        return self.op("pe", lambda g: g.matmul(out, lhsT, rhs, start=start, stop=stop), r=r, w=w)

    def tr(self, out, in_, ident, r=(), w=()):
        return self.op("pe", lambda g: g.transpose(out, in_, ident), r=r, w=w)

    def act(self, out, in_, func, r=(), w=(), e="act", **kw):
        return self.op(e, lambda g: g.activation(out=out, in_=in_, func=func, **kw), r=r, w=w)

    def tt(self, e, out, in0, in1, op, r=(), w=()):
        return self.op(e, lambda g: g.tensor_tensor(out=out, in0=in0, in1=in1, op=op), r=r, w=w)

    def ts(self, e, out, in0, s1, s2, op0, op1=None, r=(), w=()):
        if op1 is None:
            return self.op(e, lambda g: g.tensor_scalar(out=out, in0=in0, scalar1=s1, scalar2=None, op0=op0), r=r, w=w)
        return self.op(e, lambda g: g.tensor_scalar(out=out, in0=in0, scalar1=s1, scalar2=s2, op0=op0, op1=op1), r=r, w=w)

    def stt(self, out, in0, scalar, in1, op0, op1, r=(), w=()):
        return self.op("dve", lambda g: g.scalar_tensor_tensor(out=out, in0=in0, scalar=scalar, in1=in1, op0=op0, op1=op1), r=r, w=w)

    def cp(self, e, out, in_, r=(), w=()):
        if e == "act":
            return self.op(e, lambda g: g.copy(out=out, in_=in_), r=r, w=w)
        return self.op(e, lambda g: g.tensor_copy(out=out, in_=in_), r=r, w=w)

    def emit(self, final_bufs):
        waits = []
        for b in final_bufs:
            if b.lw is not None:
                waits.append((b.lw[0], b.lw[1]))
        nc = self.nc
        streams = self.stream
        with nc.Block() as block:
            def mk(name, extra):
                def body(g):
                    for ws, fn, inc in streams[name]:
                        for s, v in ws:
                            g.wait_ge(s, v)
                        if fn is not None:
                            fn(g).then_inc(inc[0], inc[1])
                    for s, v in extra:
                        g.wait_ge(s, v)
                return body
            block.sync(mk("sp", waits))
            block.scalar(mk("act", []))
            block.vector(mk("dve", []))
            block.gpsimd(mk("pool", []))
            block.tensor(mk("pe", []))


def build(debug=False):
    nc = bass.Bass("TRN2", target_bir_lowering=False)
    dt = lambda name, shape, dtype=F32, kind="ExternalInput": nc.dram_tensor(name, shape, dtype, kind=kind).ap()
    x = dt("x", [S, D])
    w_in = dt("w_in", [D, 7184])
    g1 = dt("norm1_g", [D])
    g2 = dt("norm2_g", [D])
    gf = dt("normf_g", [D])
    convw = dt("convw_l", [128, 8, 31])
    cvec = dt("cvec_l", [128, 3, 8])
    bfv = dt("fox_bf", [16])
    wco = dt("w_conv_out", [D, D])
    wfo = dt("w_fox_out", [D, D])
    wout = dt("w_out", [D, D])
    out = dt("out", [S, D], F32, "ExternalOutput")
    mixA_d = dt("mixA_d", [D, S], BF16, "Internal")
    sgB_d = dt("sgB_d", [D, S], BF16, "Internal")
    x1_d = dt("x1_d", [S, D], F32, "ExternalOutput" if debug else "Internal")
    hn2T_d = dt("hn2T_d", [NT, 128, 1024], BF16, "Internal")
    wqT = dt("wqT_l", [16, 128, 1024])
    kTd = dt("kT_l", [16, 128, 128])
    UT = dt("UT_l", [128, 128, 1024])
    Vd = dt("peer_v", [16384, 1024])
    W_d = dt("W_d", [64, 128, 128, 64], BF16, "Internal")
    CR_d = dt("CR_d", [16, 3, S], BF16, "Internal")
    WK_d = dt("WK_d", [128, 8 * 2048], BF16, "Internal")
    dbg = {}
    if debug:
        dbg["W"] = dt("dbg_W", [128, 128 * 64], BF16, "ExternalOutput")
        dbg["pe"] = dt("dbg_pe", [S, D], F32, "ExternalOutput")
        dbg["ob"] = dt("dbg_ob", [D, S], BF16, "ExternalOutput")
        dbg["y"] = dt("dbg_y", [D, S], BF16, "ExternalOutput")
        dbg["c"] = dt("dbg_c", [128, NT * 16], F32, "ExternalOutput")

    with ExitStack() as es:
        P = Prog(nc, es)
        NARENA = 105984
        arena_t = es.enter_context(nc.sbuf_tensor("arena", [128, NARENA], BF16))
        A = Arena(arena_t[:], NARENA)
        sb = lambda name, shape, dtype=F32: A.alloc(shape, dtype)
        ss = [sb("ss%d" % i, [128, 16]) for i in range(2)]
        rs = [sb("rs%d" % i, [128, 16]) for i in range(2)]
        gB = sb("gB", [128, D])
        identb = sb("identb", [128, 128], BF16)
        identf = sb("identf", [128, 128])
        onesb = sb("onesb", [128, 128], BF16)
        onesf = sb("onesf", [128, 128])
        trif = sb("trif", [128, 128])
        sel_e = sb("sel_e", [128, 128])
        sel_o = sb("sel_o", [128, 128])
        convw_s = sb("convw_s", [128, 8, 31])
        cvec_s = sb("cvec_s", [128, 3, 8])
        bfB = sb("bfB", [128, 16])
        PM0 = A.mark()
        RA = sb("RA", [128, 8, S], BF16)
        RB = sb("RB", [128, 8, S], BF16)
        PM = A.mark()

        ps = [es.enter_context(nc.psum_tensor("ps%d" % i, [128, 512], F32)) for i in range(8)]
        Bps = [Buf() for _ in range(8)]

        B = {}
        def bf(name):
            if name not in B:
                B[name] = Buf()
            return B[name]

        P.op("pool", lambda g: g.memset(onesf[:], 1.0), w=[bf("onesf")])
        P.op("pool", lambda g: g.memset(onesb[:], 1.0), w=[bf("onesb")])
        P.op("pool", lambda g: g.affine_select(out=identf[:], in_=onesf[:], pattern=[[-1, 128]], compare_op=ALU.is_equal,
                                              fill=0.0, base=0, channel_multiplier=1), r=[bf("onesf")], w=[bf("identf")])
        P.cp("pool", identb[:], identf[:], r=[bf("identf")], w=[bf("identb")])
        P.op("pool", lambda g: g.affine_select(out=trif[:], in_=onesf[:], pattern=[[1, 128]], compare_op=ALU.is_ge,
                                              fill=0.0, base=0, channel_multiplier=-1), r=[bf("onesf")], w=[bf("trif")])
        P.op("pool", lambda g: g.affine_select(out=sel_e[:], in_=onesf[:], pattern=[[0, 128]], compare_op=ALU.is_equal,
                                              fill=0.0, base=-64, channel_multiplier=1), r=[bf("onesf")], w=[bf("sel_e")])
        P.op("pool", lambda g: g.affine_select(out=sel_o[:], in_=onesf[:], pattern=[[0, 128]], compare_op=ALU.is_equal,
                                              fill=0.0, base=0, channel_multiplier=1), r=[bf("onesf")], w=[bf("sel_o")])
        P.dma("sp", gB[:], g1.partition_broadcast(128), w=[bf("gB")])
        P.dma("sp", convw_s[:], convw, w=[bf("convw")])
        P.dma("sp", cvec_s[:], cvec, w=[bf("cvec")])
        P.dma("sp", bfB[:], bfv.partition_broadcast(128), w=[bf("bfB")])

        hnT = RA
        Bxt = [Buf(), Buf()]
        Bhn = [Buf(), Buf()]
        Bss = [Buf(), Buf()]

        def rmsnorm_to_T(src_tile, srcbuf, s, i, dstT, dstbuf, psb, gtile, gbuf):
            P.act(junk[:], src_tile[:], AF.Square, r=[srcbuf], w=[bf("junk"), Bss[s]], accum_out=ss[s][:, 0:1])
            P.act(rs[s][:, 0:1], ss[s][:, 0:1], AF.Sqrt, r=[Bss[s]], w=[Bss[s]], scale=1.0 / D, bias=EPS)
            P.op("dve", lambda g: g.reciprocal(out=rs[s][:, 0:1], in_=rs[s][:, 0:1]), r=[Bss[s]], w=[Bss[s]])
            P.stt(hn[s][:], src_tile[:], rs[s][:, 0:1], gtile[:], ALU.mult, ALU.mult, r=[srcbuf, Bss[s], gbuf], w=[Bhn[s]])
            pv = ps[psb][:].bitcast(BF16)
            for k in range(8):
                P.tr(pv[:, k * 128:(k + 1) * 128], hn[s][:, k * 128:(k + 1) * 128], identb[:],
                     r=[Bhn[s], bf("identb")], w=[Bps[psb]])
            return pv

        xt = [sb("xt%d" % i, [128, D]) for i in range(2)]
        hn = [sb("hn%d" % i, [128, D], BF16) for i in range(2)]
        junk = sb("junk", [128, D], BF16)
        RBf = RB.rearrange("p k t -> p (k t)")
        WKv = RBf[:, 0:16384].rearrange("p (k n) -> p k n", k=8)
        wqs = [RBf[:, 16384 + i * 1024:16384 + (i + 1) * 1024] for i in range(2)]
        kts = [RBf[:, 18432 + i * 128:18432 + (i + 1) * 128] for i in range(2)]
        Bwq = [Buf(), Buf()]

        def wk_group(g_):
            s = g_ % 2
            P.dma("pool", wqs[s], wqT[g_], w=[Bwq[s]])
            P.dma("pool", kts[s], kTd[g_], w=[Bwq[s]])
            for kc in range(8):
                pb = 2 + (g_ * 8 + kc) // 4 % 2
                sl = (g_ * 8 + kc) % 4
                P.mm(ps[pb][:, sl * 128:(sl + 1) * 128], wqs[s][:, kc * 128:(kc + 1) * 128], kts[s], True, True,
                     r=[Bwq[s]], w=[Bps[pb]])
                if sl == 3:
                    kc0 = kc - 3
                    P.cp("dve", WKv[:, kc0:kc0 + 4, g_ * 128:(g_ + 1) * 128],
                         ps[pb][:].rearrange("p (k e) -> p k e", k=4), r=[Bps[pb]], w=[bf("WKv")])

        for i in range(NT):
            s = i % 2
            P.dma("sp", xt[s][:], x[i * 128:(i + 1) * 128, :], w=[Bxt[s]])
            pv = rmsnorm_to_T(xt[s], Bxt[s], s, i, hnT, bf("RA"), s, gB, bf("gB"))
            P.cp("act", hnT[:, :, i * 128:(i + 1) * 128], pv.rearrange("p (k t) -> p k t", k=8),
                 r=[Bps[s]], w=[bf("RA")])
            if PHASES >= 8 and i % 2 == 1:
                wk_group(i // 2)
        if PHASES >= 8:
            P.dma("sp", WK_d, WKv.rearrange("p k n -> p (k n)"), r=[bf("WKv")], w=[bf("WK_d")])

        P.barrier()
        A.release(PM)

        def load_w(dst, dbuf, src2d, col0, ncol=128):
            return P.dma("pool", dst[:, :, 0:ncol], src2d[:, col0:col0 + ncol].rearrange("(k p) j -> p k j", p=128), w=[dbuf])

        BwA = [Buf(), Buf()]
        BwB = [Buf(), Buf()]
        BwC = [Buf(), Buf()]
        Btb = [Buf() for _ in range(3)]
        Btf = [Buf() for _ in range(4)]
        yT = RB
        BY = [Buf() for _ in range(8)]
        wA = [sb("wA%d" % i, [128, 8, 128], BF16) for i in range(2)]
        wBt = [sb("wB%d" % i, [128, 8, 128], BF16) for i in range(2)]
        wC = [sb("wC%d" % i, [128, 8, 128], BF16) for i in range(2)]
        PMW = A.mark()
        tmpb = [sb("tmpb%d" % i, [128, 512], BF16) for i in range(3)]
        tmpf = [sb("tmpf%d" % i, [128, 512]) for i in range(4)]
        big = [sb("big%d" % i, [128, 4128], BF16) for i in range(2)]
        PM2 = A.mark()

        def proj(psb, wt, wbuf, src, srcbuf, tb):
            for kc in range(8):
                sbf = srcbuf[kc] if isinstance(srcbuf, list) else srcbuf
                P.mm(ps[psb][:, :], wt[:, kc, :], src[:, kc, tb * 512:(tb + 1) * 512], kc == 0, kc == 7,
                     r=[wbuf, sbf], w=[Bps[psb]])

        if PHASES >= 2:
            dgs = [sb("dg%d" % i, [128, 31, 128], BF16) for i in range(2)]
            Bdg = [Buf(), Buf()]
            for i in range(2):
                P.op("dve", lambda g, ap=big[i][:, 0:32]: g.memset(ap, 0.0), w=[bf("big%d" % i)])
            Bzc = [[Buf() for _ in range(9)] for _ in range(2)]
            it = 0

            def p2_prefetch(c):
                s = c % 2
                load_w(wA[s], BwA[s], w_in, c * 128)
                load_w(wBt[s], BwB[s], w_in, 1024 + c * 128)
                P.tt("pool", dgs[s][:], identf[:].unsqueeze(1).to_broadcast([128, 31, 128]),
                     convw_s[:, c, :].unsqueeze(2).to_broadcast([128, 31, 128]), ALU.mult,
                     r=[bf("identf"), bf("convw")], w=[Bdg[s]])

            p2_prefetch(0)
            for c in range(8):
                s = c % 2
                if c + 1 < 8:
                    p2_prefetch(c + 1)
                zc = big[s]
                dg = dgs[s]

                def p2_proj(tb):
                    pa, pb = 0 + (tb % 2) * 3, 1 + (tb % 2) * 3
                    proj(pa, wA[s], BwA[s], hnT, bf("RA"), tb)
                    proj(pb, wBt[s], BwB[s], hnT, bf("RA"), tb)
                    tbi = (c * 8 + tb) % 3
                    P.act(tmpb[tbi][:], ps[pb][:], AF.Sigmoid, r=[Bps[pb]], w=[Btb[tbi]])
                    P.tt("dve", zc[:, 30 + tb * 512: 30 + (tb + 1) * 512], ps[pa][:], tmpb[tbi][:], ALU.mult,
                         r=[Bps[pa], Btb[tbi], bf("big%d" % s)], w=[Bzc[s][tb + 1]])

                p2_proj(0)
                for tb in range(8):
                    if tb + 1 < 8:
                        p2_proj(tb + 1)
                    py = 2 + (tb % 2) * 3
                    for w_ in range(31):
                        P.mm(ps[py][:], dg[:, w_, :], zc[:, tb * 512 + w_: tb * 512 + w_ + 512], w_ == 0, w_ == 30,
                             r=[Bdg[s], Bzc[s][tb], Bzc[s][tb + 1], bf("big%d" % s)], w=[Bps[py]])
                    P.act(yT[:, c, tb * 512:(tb + 1) * 512], ps[py][:], AF.Identity, r=[Bps[py], bf("cvec")], w=[BY[c]],
                          bias=cvec_s[:, 0, c:c + 1])
            if debug:
                for c in range(8):
                    P.dma("sp", dbg["y"][c * 128:(c + 1) * 128, :], yT[:, c, :], r=[BY[c]], w=[bf("dbg_y")])

        P.barrier()
        A.release(PM2)
        if PHASES >= 3:
            ub = [sb("ub%d" % i, [128, 512]) for i in range(3)]
            u2 = [sb("u2%d" % i, [128, 512]) for i in range(3)]
            Bub = [Buf() for _ in range(3)]
            Bu2 = [Buf() for _ in range(3)]
            for tb in range(8):
                blk = slice(tb * 512, (tb + 1) * 512)
                for c in range(8):
                    tbi = c % 3
                    P.act(tmpb[tbi][:], yT[:, c, blk], AF.Square, r=[BY[c]], w=[Btb[tbi]])
                    P.mm(ps[0][:], onesb[:], yT[:, c, blk], c == 0, c == 7, r=[bf("onesb"), BY[c]], w=[Bps[0]])
                    P.mm(ps[1][:], onesb[:], tmpb[tbi][:], c == 0, c == 7, r=[bf("onesb"), Btb[tbi]], w=[Bps[1]])
                mean, msq, var, nmr = tmpf[0], tmpf[1], tmpf[2], tmpf[3]
                P.ts("dve", mean[:], ps[0][:], 1.0 / 1024, None, ALU.mult, r=[Bps[0]], w=[Btf[0]])
                P.tt("dve", msq[:], mean[:], mean[:], ALU.mult, r=[Btf[0]], w=[Btf[1]])
                P.stt(var[:], ps[1][:], 1.0 / 1024, msq[:], ALU.mult, ALU.subtract, r=[Bps[1], Btf[1]], w=[Btf[2]])
                P.act(var[:], var[:], AF.Sqrt, r=[Btf[2]], w=[Btf[2]], bias=EPS, scale=1.0)
                P.op("dve", lambda g, var=var: g.reciprocal(out=var[:], in_=var[:]), r=[Btf[2]], w=[Btf[2]])
                P.stt(nmr[:], mean[:], -1.0, var[:], ALU.mult, ALU.mult, r=[Btf[0], Btf[2]], w=[Btf[3]])
                for c in range(8):
                    k = c % 3
                    P.tt("dve", ub[k][:], yT[:, c, blk], var[:], ALU.mult, r=[BY[c], Btf[2]], w=[Bub[k]])
                    P.tt("pool", u2[k][:], ub[k][:], nmr[:], ALU.add, r=[Bub[k], Btf[3]], w=[Bu2[k]])
                    P.act(yT[:, c, blk], u2[k][:], AF.Silu, r=[Bu2[k], bf("cvec")], w=[BY[c]],
                          scale=cvec_s[:, 1, c:c + 1], bias=cvec_s[:, 2, c:c + 1])

        if PHASES >= 4:
            it = 0
            for n in range(8):
                s = n % 2
                load_w(wA[s], BwA[s], wco, n * 128)
                load_w(wBt[s], BwB[s], w_in, 5136 + n * 128)
                load_w(wC[s], BwC[s], w_in, 6160 + n * 128)
                for tb in range(8):
                    blk = slice(tb * 512, (tb + 1) * 512)
                    o = (tb % 2) * 3
                    proj(o + 0, wA[s], BwA[s], yT, BY, tb)
                    proj(o + 1, wBt[s], BwB[s], hnT, bf("RA"), tb)
                    proj(o + 2, wC[s], BwC[s], hnT, bf("RA"), tb)
                    tbi = it % 3
                    it += 1
                    P.act(tmpb[tbi][:], ps[o + 1][:], AF.Sigmoid, r=[Bps[o + 1]], w=[Btb[tbi]])
                    P.tt("dve", big[0][:, blk], ps[o + 0][:], tmpb[tbi][:], ALU.mult, r=[Bps[o + 0], Btb[tbi]], w=[bf("big0")])
                    P.act(big[1][:, blk], ps[o + 2][:], AF.Sigmoid, r=[Bps[o + 2]], w=[bf("big1")])
                P.dma("sp", mixA_d[n * 128:(n + 1) * 128, :], big[0][:, 0:S], r=[bf("big0")], w=[bf("mixA_d")])
                P.dma("sp", sgB_d[n * 128:(n + 1) * 128, :], big[1][:, 0:S], r=[bf("big1")], w=[bf("sgB_d")])

        P.barrier()
        A.release(PMW)
        if PHASES >= 5:
            fb = sb("fb", [128, NT, 16])
            Cc = sb("Cc", [128, NT, 16])
            crefB = sb("crefB", [128, 16, 16])
            qT = [sb("qT%d" % i, [128, S], BF16) for i in range(2)]
            kTa = [sb("kT%d" % i, [128, S], BF16) for i in range(2)]
            cres = [fb, Cc]
            Vaug = sb("Vaug", [128, NT, 193], BF16)
            Rz_e = sb("Rz_e", [128, 256])
            Rz_o = sb("Rz_o", [128, 256])
            rb = [sb("rb%d" % i, [128, 256]) for i in range(2)]
            PM5 = A.mark()
            wf_s = sb("wf_s", [128, 8, 16], BF16)
            incl = fb
            raw12 = sb("raw12", [128, 2 * NT * 16], BF16)
            Tsb = raw12.bitcast(F32).rearrange("p (i h) -> p i h", h=16)
            csp = [sb("csp0", [128, NT, 16], BF16), raw12[:, 0:NT * 16].rearrange("p (i h) -> p i h", h=16),
                   raw12[:, NT * 16:2 * NT * 16].rearrange("p (i h) -> p i h", h=16)]
            cstg1 = sb("cstg", [16, 384], BF16)
            cstg = [cstg1, cstg1]
            P.op("pool", lambda g: g.memset(Rz_e[:], 0.0), w=[bf("Rz_e")])
            P.op("pool", lambda g: g.memset(Rz_o[:], 0.0), w=[bf("Rz_o")])
            P.op("pool", lambda g: g.memset(qT[0][64:128, :], 0.0), w=[bf("qT")])
            P.op("pool", lambda g: g.memset(qT[1][0:64, :], 0.0), w=[bf("qT")])
            P.op("pool", lambda g: g.memset(qT[0][64:68, :], 8.0), w=[bf("qT")])
            P.op("pool", lambda g: g.memset(qT[1][0:4, :], 8.0), w=[bf("qT")])
            P.op("pool", lambda g: g.memset(kTa[0][64:128, :], 0.0), w=[bf("kT")])
            P.op("pool", lambda g: g.memset(kTa[1][0:64, :], 0.0), w=[bf("kT")])
            P.op("pool", lambda g: g.memset(kTa[0][64:65, :], 1.0), w=[bf("kT")])
            P.op("pool", lambda g: g.memset(kTa[1][0:1, :], 1.0), w=[bf("kT")])
            P.op("pool", lambda g: g.memset(Vaug[:], 0.0), w=[bf("Vaug")])
            P.op("pool", lambda g: g.memset(Vaug[:, :, 64:66], 1.0), w=[bf("Vaug")])
            P.dma("pool", wf_s[:], w_in[:, 5120:5136].rearrange("(k p) j -> p k j", p=128), w=[bf("wf")])
            for i in range(NT):
                for kc in range(8):
                    P.mm(ps[0][:, i * 16:(i + 1) * 16], hnT[:, kc, i * 128:(i + 1) * 128], wf_s[:, kc, :], kc == 0, kc == 7,
                         r=[bf("RA"), bf("wf")], w=[Bps[0]])
            P.tt("dve", fb[:], ps[0][:].rearrange("p (i h) -> p i h", h=16), bfB[:].unsqueeze(1).to_broadcast([128, NT, 16]),
                 ALU.add, r=[Bps[0], bf("bfB")], w=[bf("fb")])
            P.act(fb[:], fb[:], AF.Exp, r=[bf("fb")], w=[bf("fb")], scale=-1.0)
            P.act(fb[:], fb[:], AF.Ln, r=[bf("fb")], w=[bf("fb")], bias=1.0, scale=1.0)
            P.ts("dve", fb[:], fb[:], -1.0, None, ALU.mult, r=[bf("fb")], w=[bf("fb")])
            for i in range(NT):
                P.mm(ps[1][:, i * 16:(i + 1) * 16], trif[:], fb[:, i, :], True, True, r=[bf("trif"), bf("fb")], w=[Bps[1]])
            for i in range(NT):
                P.mm(ps[2][:, i * 16:(i + 1) * 16], onesf[:], fb[:, i, :], True, True, r=[bf("onesf"), bf("fb")], w=[Bps[2]])
            P.cp("dve", Tsb[:], ps[2][:].rearrange("p (i h) -> p i h", h=16), r=[Bps[2]], w=[bf("csp")])
            for h in range(16):
                P.op("dve", lambda g, o=incl[:, :, h], d0=onesf[:, 0:NT], d1=Tsb[:, :, h]: g.tensor_tensor_scan(
                    out=o, data0=d0, data1=d1, initial=0.0, op0=ALU.mult, op1=ALU.add),
                    r=[bf("csp"), bf("onesf")], w=[bf("fb")])
            P.cp("dve", crefB[:], incl[:].rearrange("p (q two) h -> p q two h", two=2)[:, :, 0, :], r=[bf("fb")], w=[bf("crefB")])
            P.tt("dve", Tsb[:], incl[:], Tsb[:], ALU.subtract, r=[bf("fb"), bf("csp")], w=[bf("csp")])
            P.tt("dve", Cc[:], ps[1][:].rearrange("p (i h) -> p i h", h=16), Tsb[:], ALU.add, r=[Bps[1], bf("csp")], w=[bf("Cc")])
            if debug:
                P.dma("sp", dbg["c"], Cc[:].rearrange("p i h -> p (i h)"), r=[bf("Cc")], w=[bf("dbg_c")])
            P.ts("dve", cres[0][:], Cc[:], -1.0, None, ALU.mult, r=[bf("Cc")], w=[bf("fb")])
            P.cp("dve", csp[0][:], cres[0][:], r=[bf("fb")], w=[bf("csp")])
            P.tt("dve", cres[1][:], cres[0][:], csp[0][:], ALU.subtract, r=[bf("fb"), bf("csp")], w=[bf("Cc")])
            P.cp("dve", csp[1][:], cres[1][:], r=[bf("Cc")], w=[bf("csp")])
            P.tt("dve", cres[0][:], cres[1][:], csp[1][:], ALU.subtract, r=[bf("Cc"), bf("csp")], w=[bf("fb")])
            P.cp("dve", csp[2][:], cres[0][:], r=[bf("fb")], w=[bf("csp")])
            Bc1 = Buf()
            Bcst = [Bc1, Bc1]
            for i in range(NT):
                k = i % 2
                pvc = ps[3 + k][:].bitcast(BF16)
                for r_ in range(3):
                    P.tr(pvc[0:16, r_ * 128:(r_ + 1) * 128], csp[r_][:, i, :], identb[:], r=[bf("csp"), bf("identb")], w=[Bps[3 + k]])
                P.cp("act", cstg[k][0:16, :], pvc[0:16, 0:384], r=[Bps[3 + k]], w=[Bcst[k]])
                P.dma("sp", CR_d[:, :, i * 128:(i + 1) * 128], cstg[k][0:16, :].rearrange("p (r t) -> p r t", r=3), r=[Bcst[k]], w=[bf("CR_d")])

            P.barrier()
            A.release(PM5)
            pT = [sb("pT%d" % i, [128, 512], BF16) for i in range(4)]
            obT = RB
            BpT = [Buf() for _ in range(4)]
            Brb = [Buf(), Buf()]
            ipt = 0
            def p5_loads(hp):
                s = hp % 2
                load_w(wA[s], BwA[s], w_in, 2048 + hp * 128)
                load_w(wBt[s], BwB[s], w_in, 3072 + hp * 128)
                load_w(wC[s], BwC[s], w_in, 4096 + hp * 128)

            p5_loads(0)
            for hp in range(8):
                s = hp % 2
                P.dma("sp", kTa[0][65:68, :], CR_d[2 * hp], r=[bf("CR_d")], w=[bf("kT")])
                P.dma("sp", kTa[1][1:4, :], CR_d[2 * hp + 1], r=[bf("CR_d")], w=[bf("kT")])
                P.ts("dve", qT[0][64:65, :].rearrange("p (q c) -> p q c", c=256),
                     crefB[64:65, :, 2 * hp:2 * hp + 1].to_broadcast([1, 16, 256]), 8.0, None, ALU.mult, r=[bf("crefB")], w=[bf("qT")])
                P.ts("dve", qT[1][0:1, :].rearrange("p (q c) -> p q c", c=256),
                     crefB[0:1, :, 2 * hp + 1:2 * hp + 2].to_broadcast([1, 16, 256]), 8.0, None, ALU.mult, r=[bf("crefB")], w=[bf("qT")])
                for tb in range(8):
                    blk = slice(tb * 512, (tb + 1) * 512)
                    proj(0 + tb % 2, wA[s], BwA[s], hnT, bf("RA"), tb)
                    P.cp("act", qT[0][0:64, blk], ps[0 + tb % 2][0:64, :], r=[Bps[0 + tb % 2]], w=[bf("qT")])
                    P.cp("pool" if False else "dve", qT[1][64:128, blk], ps[0 + tb % 2][64:128, :], r=[Bps[0 + tb % 2]], w=[bf("qT")])
                    proj(2 + tb % 2, wBt[s], BwB[s], hnT, bf("RA"), tb)
                    P.cp("dve", kTa[0][0:64, blk], ps[2 + tb % 2][0:64, :], r=[Bps[2 + tb % 2]], w=[bf("kT")])
                    P.cp("dve", kTa[1][64:128, blk], ps[2 + tb % 2][64:128, :], r=[Bps[2 + tb % 2]], w=[bf("kT")])
                for i4 in range(8):
                    pb = 4 + i4 % 2
                    for ii in range(4):
                        i = i4 * 4 + ii
                        for kc in range(8):
                            P.mm(ps[pb][:, ii * 128:(ii + 1) * 128], hnT[:, kc, i * 128:(i + 1) * 128], wC[s][:, kc, :], kc == 0, kc == 7,
                                 r=[bf("RA"), BwC[s]], w=[Bps[pb]])
                    pv3 = ps[pb][:].rearrange("p (i c) -> p i c", c=128)
                    P.cp("act", Vaug[:, i4 * 4:(i4 + 1) * 4, 0:64], pv3[:, :, 0:64], r=[Bps[pb]], w=[bf("Vaug")])
                    P.cp("dve", Vaug[:, i4 * 4:(i4 + 1) * 4, 129:193], pv3[:, :, 64:128], r=[Bps[pb]], w=[bf("Vaug")])
                if hp + 1 < 8:
                    p5_loads(hp + 1)
                items = [(e_, Q, kt) for e_ in range(2) for Q in range(8) for kt in range(4 * Q + 4)]
                LA = 3

                def geom(Q, kt):
                    c0 = max(0, kt - 4 * Q) * 128
                    return c0, 512 - c0

                def s_exp(n):
                    e_, Q, kt = items[n]
                    c0, ncol = geom(Q, kt)
                    q0 = Q * 512 + c0
                    psb = n % 4
                    P.mm(ps[psb][:, 0:ncol], kTa[e_][:, kt * 128:(kt + 1) * 128], qT[e_][:, q0:q0 + ncol],
                         True, True, r=[bf("kT"), bf("qT")], w=[Bps[psb]])
                    P.act(pT[psb][:, 0:ncol], ps[psb][:, 0:ncol], AF.Exp, r=[Bps[psb]], w=[BpT[psb]], scale=0.125)
                    if kt >= 4 * Q:
                        P.op("pool", lambda g, a=pT[psb][:, 0:128]: g.affine_select(out=a, in_=a, pattern=[[1, 128]],
                                                                                     compare_op=ALU.is_ge, fill=0.0, base=0, channel_multiplier=-1),
                             r=[BpT[psb]], w=[BpT[psb]])

                def pv(n):
                    e_, Q, kt = items[n]
                    c0, ncol = geom(Q, kt)
                    po = 6 + Q % 2
                    nk = 4 * Q + 4
                    pi = n % 4
                    if e_ == 0:
                        P.mm(ps[po][0:65, c0:c0 + ncol], Vaug[:, kt, 0:65], pT[pi][:, 0:ncol], kt == 0, kt == nk - 1,
                             r=[bf("Vaug"), BpT[pi]], w=[Bps[po]])
                    else:
                        P.mm(ps[po][:, c0:c0 + ncol], Vaug[:, kt, 65:193], pT[pi][:, 0:ncol], kt == 0, kt == nk - 1,
                             r=[bf("Vaug"), BpT[pi]], w=[Bps[po]])
                    return kt == nk - 1

                def fin_a(e_, Q, half):
                    po = 6 + Q % 2
                    Rz, rrow = (Rz_e, 64) if e_ == 0 else (Rz_o, 0)
                    Bz = bf("Rz_e") if e_ == 0 else bf("Rz_o")
                    cs = slice(half * 256, (half + 1) * 256)
                    P.op("dve", lambda g, o=Rz[rrow:rrow + 1, :], a=ps[po][rrow:rrow + 1, cs]: g.reciprocal(out=o, in_=a),
                         r=[Bps[po]], w=[Bz])

                def fin_b(e_, Q, half):
                    po = 6 + Q % 2
                    pbase = 64 * e_
                    Rz, sel = (Rz_e, sel_e) if e_ == 0 else (Rz_o, sel_o)
                    Bz = bf("Rz_e") if e_ == 0 else bf("Rz_o")
                    pbc = 4 + half
                    cs = slice(half * 256, (half + 1) * 256)
                    P.mm(ps[pbc][:, 0:256], sel[:], Rz[:], True, True, r=[bf("sel_e"), bf("sel_o"), Bz], w=[Bps[pbc]])
                    ri = half
                    P.cp("dve", rb[ri][:], ps[pbc][:, 0:256], r=[Bps[pbc]], w=[Brb[ri]])
                    P.tt("dve", obT[pbase:pbase + 64, hp, Q * 512 + half * 256:Q * 512 + (half + 1) * 256], ps[po][pbase:pbase + 64, cs],
                         rb[ri][pbase:pbase + 64, :], ALU.mult, r=[Bps[po], Brb[ri]], w=[bf("RB")])

                pend = []
                for n in range(len(items) + LA):
                    if n < len(items):
                        s_exp(n)
                    for pe_ in pend:
                        pe_[0] -= 1
                    while pend and pend[0][0] <= 0:
                        _, e2_, q2_, h2_ = pend.pop(0)
                        fin_b(e2_, q2_, h2_)
                        if h2_ == 0:
                            fin_a(e2_, q2_, 1)
                            pend.insert(0, [6, e2_, q2_, 1])
                    if n >= LA:
                        if pv(n - LA):
                            while pend:
                                _, e3_, q3_, h3_ = pend.pop(0)
                                fin_b(e3_, q3_, h3_)
                                if h3_ == 0:
                                    fin_a(e3_, q3_, 1)
                                    fin_b(e3_, q3_, 1)
                            e2_, q2_, _ = items[n - LA]
                            fin_a(e2_, q2_, 0)
                            pend.append([6, e2_, q2_, 0])
                while pend:
                    _, e3_, q3_, h3_ = pend.pop(0)
                    fin_b(e3_, q3_, h3_)
                    if h3_ == 0:
                        fin_a(e3_, q3_, 1)
                        fin_b(e3_, q3_, 1)
            if debug:
                for c in range(8):
                    P.dma("sp", dbg["ob"][c * 128:(c + 1) * 128, :], obT[:, c, :], r=[bf("RB")], w=[bf("dbg_ob")])

        P.barrier()
        A.release(PMW)
        if PHASES >= 6:
            tmpf = [sb("tmpf%d" % i, [128, 512]) for i in range(4)]
            big = [sb("big%d" % i, [128, 4128], BF16) for i in range(2)]
            mixinT = RA
            for n in range(8):
                s = n % 2
                load_w(wA[s], BwA[s], wfo, n * 128)
                P.dma("sp", big[0][:, 0:S], mixA_d[n * 128:(n + 1) * 128, :], r=[bf("mixA_d")], w=[bf("big0")])
                P.dma("sp", big[1][:, 0:S], sgB_d[n * 128:(n + 1) * 128, :], r=[bf("sgB_d")], w=[bf("big1")])
                for tb in range(8):
                    blk = slice(tb * 512, (tb + 1) * 512)
                    pb = tb % 2
                    proj(pb, wA[s], BwA[s], obT, bf("RB"), tb)
                    ti = tb % 2
                    P.tt("dve", tmpf[ti][:], ps[pb][:], big[1][:, blk], ALU.mult, r=[Bps[pb], bf("big1")], w=[Btf[ti]])
                    P.tt("pool" if tb % 2 == 0 else "dve", mixinT[:, n, blk], tmpf[ti][:], big[0][:, blk], ALU.add,
                         r=[Btf[ti], bf("big0")], w=[bf("RA")])

            P.barrier()
            A.release(PM)
            woutS = sb("woutS", [128, 8, D], BF16)
            hn = [sb("hn%d" % i, [128, D], BF16) for i in range(2)]
            junk = sb("junk", [128, D], BF16)
            xt = [sb("xt%d" % i, [128, D]) for i in range(2)]
            x1t = [sb("x1t%d" % i, [128, D]) for i in range(2)]
            hst = [sb("hst%d" % i, [128, 1024], BF16) for i in range(2)]
            P.dma("pool", woutS[:], wout.rearrange("(k p) n -> p k n", p=128), w=[bf("wout")])
            P.dma("sp", gB[:], g2.partition_broadcast(128), r=[bf("gB")], w=[bf("gB")])
            Bx1 = [Buf(), Buf()]
            Bhst = [Buf(), Buf()]
            def p7_mm(i):
                s = i % 2
                P.dma("sp", xt[s][:], x[i * 128:(i + 1) * 128, :], w=[Bxt[s]])
                for half in range(2):
                    pb = 2 * s + half
                    for kc in range(8):
                        P.mm(ps[pb][:], mixinT[:, kc, i * 128:(i + 1) * 128], woutS[:, kc, half * 512:(half + 1) * 512], kc == 0, kc == 7,
                             r=[bf("RA"), bf("wout")], w=[Bps[pb]])
                    P.tt("dve", x1t[s][:, half * 512:(half + 1) * 512], ps[pb][:], xt[s][:, half * 512:(half + 1) * 512], ALU.add,
                         r=[Bps[pb], Bxt[s]], w=[Bx1[s]])
                P.dma("sp", x1_d[i * 128:(i + 1) * 128, :], x1t[s][:], r=[Bx1[s]], w=[bf("x1_d")])

            def p7_norm(i):
                s = i % 2
                pv = rmsnorm_to_T(x1t[s], Bx1[s], s, i, None, None, 4 + s, gB, bf("gB"))
                P.cp("act", hst[s][:], pv, r=[Bps[4 + s]], w=[Bhst[s]])
                P.dma("sp", hn2T_d[i], hst[s][:], r=[Bhst[s]], w=[bf("hn2T_d")])

            p7_mm(0)
            for i in range(NT):
                if i + 1 < NT:
                    p7_mm(i + 1)
                p7_norm(i)

        NTS = int(os.environ.get("MK_NTS", str(NT)))
        if PHASES >= 8:
            P.barrier()
            A.release(PM0)
            WK = sb("WK", [128, 8, 2048], BF16)
            P.dma("sp", WK[:].rearrange("p k n -> p (k n)"), WK_d, r=[bf("WK_d")], w=[bf("WK")])
            AX = mybir.AxisListType
            hst1 = sb("hst", [128, 1024], BF16)
            hst = [hst1, hst1]
            Ssb1 = sb("Ssb", [128, 16, 128])
            Ssb = [Ssb1, Ssb1]
            Vt = [sb("Vt%d" % i, [128, 16, 16]) for i in range(2)]
            SC = [sb("SC%d" % i, [128, 8, 16]) for i in range(2)]
            negm = [sb("negm%d" % i, [128, 8]) for i in range(2)]
            Zs = [sb("Zs%d" % i, [128, 8]) for i in range(2)]
            nbias = [sb("nbias%d" % i, [128, 8]) for i in range(2)]
            coefE = [sb("coefE%d" % i, [128, 8]) for i in range(2)]
            ed = sb("ed", [128, 8, 16])
            Stmp = sb("Stmp", [128, 128])
            cand = sb("cand", [128, 8, 256])
            candt = sb("candt", [128, 256])
            m1 = sb("m1", [128, 4, 128])
            S1m = m1
            sig = sb("sig", [128, 4, 7, 128])
            Es = sb("Es", [128, 4, 7, 128], BF16)

            def dual(n_bf16, pat, **kw):
                raw = sb("raw", [128, n_bf16], BF16)
                return raw.rearrange(pat[0], **kw), raw.bitcast(F32).rearrange(pat[1], **kw)
            A1d = [dual(8 * 7 * 128, ("p (h r e) -> p h r e", "p (h r e) -> p h r e"), h=8, r=7) for _ in range(2)]
            A2d = [dual(8 * 7 * 128, ("p (h r e) -> p h r e", "p (h r e) -> p h r e"), h=8, r=7) for _ in range(2)]
            A1T = sb("A1T", [128, 64, 64, 2], BF16)
            A2T = sb("A2T", [128, 128, 64], BF16)
            A2Tq = A2T.rearrange("p (q r) t -> p q r t", r=2)
            WTs2 = [sb("WTs%d" % i, [128, 128, 64], BF16) for i in range(2)]
            BWT = [Buf(), Buf()]
            Bh1 = Buf()
            Bhst = [Bh1, Bh1]
            BS1 = Buf()
            BS = [BS1, BS1]
            BV = [Buf(), Buf()]
            BSC = [Buf(), Buf()]
            Bst = [Buf(), Buf()]
            BA1 = [Buf(), Buf()]
            BA2 = [Buf(), Buf()]
            NEG = -1.0e30
            NJ = 56

            def xa_s(i):
                s = i % 2
                P.dma("sp", hst[s][:], hn2T_d[i], r=[bf("hn2T_d")], w=[Bhst[s]])
                for q4 in range(4):
                    for kc in range(8):
                        P.mm(ps[q4][:], hst[s][:, kc * 128:(kc + 1) * 128], WK[:, kc, q4 * 512:(q4 + 1) * 512], kc == 0, kc == 7,
                             r=[Bhst[s], bf("WK")], w=[Bps[q4]])
                    P.cp("dve", Ssb[s][:, q4 * 4:(q4 + 1) * 4, :], ps[q4][:].rearrange("p (g e) -> p g e", g=4),
                         r=[Bps[q4]], w=[BS[s]])

            def xa_rest(i):
                s = i % 2
                for g_ in range(16):
                    P.op("dve", lambda g, o=Vt[s][:, g_, 0:8], a=Ssb[s][:, g_, :]: g.max(out=o, in_=a), r=[BS[s]], w=[BV[s]])
                    P.op("dve", lambda g, o=Stmp[:], v=Vt[s][:, g_, 0:8], a=Ssb[s][:, g_, :]: g.match_replace(out=o, in_to_replace=v, in_values=a, imm_value=NEG),
                         r=[BS[s], BV[s]], w=[bf("Stmp")])
                    P.op("dve", lambda g, o=Vt[s][:, g_, 8:16], a=Stmp[:]: g.max(out=o, in_=a), r=[bf("Stmp")], w=[BV[s]])
                P.tt("pool", cand[:].rearrange("p h (a b) -> p h a b", a=16), Vt[s][:, 0:8, :].unsqueeze(3).to_broadcast([128, 8, 16, 16]),
                     Vt[s][:, 8:16, :].unsqueeze(2).to_broadcast([128, 8, 16, 16]), ALU.add, r=[BV[s]], w=[bf("cand")])
                for h in range(8):
                    P.op("dve", lambda g, o=SC[s][:, h, 0:8], a=cand[:, h, :]: g.max(out=o, in_=a), r=[bf("cand")], w=[BSC[s]])
                    P.op("dve", lambda g, o=candt[:], v=SC[s][:, h, 0:8], a=cand[:, h, :]: g.match_replace(out=o, in_to_replace=v, in_values=a, imm_value=NEG),
                         r=[bf("cand"), BSC[s]], w=[bf("candt")])
                    P.op("dve", lambda g, o=SC[s][:, h, 8:16], a=candt[:]: g.max(out=o, in_=a), r=[bf("candt")], w=[BSC[s]])

            def xa_stats(i):
                s = i % 2
                P.ts("dve", negm[s][:], SC[s][:, :, 0], -1.0, None, ALU.mult, r=[BSC[s]], w=[Bst[s]])
                P.tt("dve", ed[:], SC[s][:], negm[s][:].unsqueeze(2).to_broadcast([128, 8, 16]), ALU.add, r=[BSC[s], Bst[s]], w=[bf("ed")])
                P.act(ed[:], ed[:], AF.Exp, r=[bf("ed")], w=[bf("ed")])
                P.op("dve", lambda g, o=Zs[s][:], a=ed[:]: g.tensor_reduce(out=o, in_=a, axis=AX.X, op=ALU.add), r=[bf("ed")], w=[Bst[s]])
                P.act(nbias[s][:], Zs[s][:], AF.Ln, r=[Bst[s]], w=[Bst[s]])
                P.tt("dve", nbias[s][:], negm[s][:], nbias[s][:], ALU.subtract, r=[Bst[s]], w=[Bst[s]])
                P.act(coefE[s][:], nbias[s][:], AF.Exp, r=[Bst[s]], w=[Bst[s]])

            def xb(i, hh):
                s = i % 2
                A1, A2 = A1d[s][0], A2d[s][0]
                hs = slice(4 * hh, 4 * hh + 4)
                h2 = slice(8 + 4 * hh, 8 + 4 * hh + 4)
                P.tt("pool", sig[:, :, 0:4, :], Ssb[s][:, h2, :].unsqueeze(2).to_broadcast([128, 4, 4, 128]),
                     Vt[s][:, hs, 0:4].unsqueeze(3).to_broadcast([128, 4, 4, 128]), ALU.add, r=[BS[s], BV[s]], w=[bf("sig")])
                P.tt("dve", m1[:], Ssb[s][:, hs, :], Vt[s][:, hs, 3:4].to_broadcast([128, 4, 128]), ALU.is_ge, r=[BS[s], BV[s]], w=[bf("m1")])
                P.stt(S1m[:], m1[:], NEG, Ssb[s][:, hs, :], ALU.mult, ALU.add, r=[BS[s]], w=[bf("m1")])
                P.tt("pool", sig[:, :, 4:7, :], S1m[:].unsqueeze(2).to_broadcast([128, 4, 3, 128]),
                     Vt[s][:, h2, 0:3].unsqueeze(3).to_broadcast([128, 4, 3, 128]), ALU.add, r=[bf("m1"), BV[s]], w=[bf("sig")])
                P.act(Es[:], sig[:], AF.Exp, r=[bf("sig")], w=[bf("Es")])
                P.tt("dve", A1[:, hs, 0:4, :], Ssb[s][:, hs, :].unsqueeze(2).to_broadcast([128, 4, 4, 128]),
                     Vt[s][:, hs, 0:4].unsqueeze(3).to_broadcast([128, 4, 4, 128]), ALU.is_equal, r=[BS[s], BV[s]], w=[BA1[s]])
                P.tt("pool", A1[:, hs, 0:4, :], A1[:, hs, 0:4, :], coefE[s][:, hs].unsqueeze(2).unsqueeze(3).to_broadcast([128, 4, 4, 128]),
                     ALU.mult, r=[Bst[s]], w=[BA1[s]])
                P.tt("dve", A2[:, hs, 4:7, :], Ssb[s][:, h2, :].unsqueeze(2).to_broadcast([128, 4, 3, 128]),
                     Vt[s][:, h2, 0:3].unsqueeze(3).to_broadcast([128, 4, 3, 128]), ALU.is_equal, r=[BS[s], BV[s]], w=[BA2[s]])
                P.tt("pool", A2[:, hs, 4:7, :], A2[:, hs, 4:7, :], coefE[s][:, hs].unsqueeze(2).unsqueeze(3).to_broadcast([128, 4, 3, 128]),
                     ALU.mult, r=[Bst[s]], w=[BA2[s]])
                for hl in range(4):
                    h = 4 * hh + hl
                    P.stt(A2[:, h, 0:4, :], sig[:, hl, 0:4, :], SC[s][:, h, 15:16], Es[:, hl, 0:4, :], ALU.is_ge, ALU.mult,
                          r=[bf("sig"), bf("Es"), BSC[s]], w=[BA2[s]])
                    P.stt(A1[:, h, 4:7, :], sig[:, hl, 4:7, :], SC[s][:, h, 15:16], Es[:, hl, 4:7, :], ALU.is_ge, ALU.mult,
                          r=[bf("sig"), bf("Es"), BSC[s]], w=[BA1[s]])

            def y_tr(i, hf):
                s = i % 2
                p0 = hf * 64
                for side, (Asrc, nm, bA) in enumerate(((A1d[s][1], "A1", BA1[s]), (A2d[s][1], "A2", BA2[s]))):
                    for e8 in range(8):
                        pb = 4 + (e8 % 2) + 2 * side
                        for ee in range(8):
                            ep = e8 * 8 + ee
                            P.tr(ps[pb][0:NJ, ee * 64:(ee + 1) * 64], Asrc[p0:p0 + 64, :, :, ep].rearrange("p h a -> p (h a)"),
                                 identf[p0:p0 + 64, p0:p0 + 64], r=[bA, bf("identf")], w=[Bps[pb]])
                        pvb = ps[pb][:].bitcast(BF16)
                        if side == 0:
                            P.cp("act", A1T[0:NJ, e8 * 8:(e8 + 1) * 8, :, :].rearrange("p e t r -> p (e t r)"), pvb[0:NJ, :],
                                 r=[Bps[pb]], w=[bf("A1T")])
                        else:
                            P.cp("act", A2Tq[0:NJ, e8 * 8:(e8 + 1) * 8, :, :], pvb[0:NJ, :].rearrange("p (e t r) -> p e r t", e=8, r=2),
                                 r=[Bps[pb]], w=[bf("A2T")])

            def y_w(i, hf):
                WTs = WTs2[hf]
                bW = BWT[hf]
                for t4 in range(16):
                    pb = t4 % 4
                    for tt_ in range(4):
                        t = t4 * 4 + tt_
                        P.mm(ps[pb][:, tt_ * 128:(tt_ + 1) * 128], A2T[0:NJ, :, t], A1T[0:NJ, :, t, :], True, True,
                             r=[bf("A1T"), bf("A2T")], w=[Bps[pb]])
                    P.cp("act", WTs[:, :, t4 * 4:(t4 + 1) * 4], ps[pb][:].rearrange("p (t e) -> p e t", t=4),
                         r=[Bps[pb]], w=[bW])
                P.dma("sp", W_d[i * 2 + hf], WTs[:], r=[bW], w=[bf("W_d")])
                if debug and i == 0 and hf == 0:
                    P.dma("sp", dbg["W"], WTs[:].rearrange("p e t -> p (e t)"), r=[bW], w=[bf("dbg_W")])

            xa_s(0)
            xa_rest(0)
            xa_stats(0)
            xb(0, 0)
            xb(0, 1)
            for i in range(NTS):
                nxt = i + 1 < NTS
                if nxt:
                    xa_s(i + 1)
                y_tr(i, 0)
                if nxt:
                    xa_rest(i + 1)
                y_w(i, 0)
                y_tr(i, 1)
                if nxt:
                    xa_stats(i + 1)
                    xb(i + 1, 0)
                y_w(i, 1)
                if nxt:
                    xb(i + 1, 1)

        if PHASES >= 9:
            P.barrier()
            A.release(PM0)
            GELU = getattr(AF, os.environ.get("MK_GELU", "Gelu_apprx_tanh"))
            hn2Tb = sb("hn2Tb", [128, 8, 1024], BF16)
            junk = sb("junk", [128, D], BF16)
            acc = sb("acc", [128, 8, 1024])
            Ub = [sb("Ub%d" % i, [128, 8, 128], BF16) for i in range(3)]
            Vb = [sb("Vb%d" % i, [128, 8, 1024], BF16) for i in range(2)]
            Wblk = [sb("Wblk%d" % i, [128, 16, 8, 64], BF16) for i in range(2)]
            gel = [sb("gel%d" % i, [128, 512], BF16) for i in range(3)]
            Gg = [sb("Gg%d" % i, [128, 8, 1024], BF16) for i in range(2)]
            xt = [sb("xt%d" % i, [128, D]) for i in range(2)]
            x2 = [sb("x2%d" % i, [128, D]) for i in range(2)]
            ot = [sb("ot%d" % i, [128, D]) for i in range(2)]
            P.dma("sp", gB[:], gf.partition_broadcast(128), r=[bf("gB")], w=[bf("gB")])
            BU = [Buf() for _ in range(3)]
            BV = [Buf(), Buf()]
            BW = [Buf(), Buf()]
            Bgel = [Buf() for _ in range(3)]
            BG = [Buf(), Buf()]
            Bx2 = [Buf(), Buf()]
            Bot = [Buf(), Buf()]
            Bacc = [Buf() for _ in range(16)]
            def load_hn2Tb(Bk):
                for ti in range(8):
                    P.dma("sp", hn2Tb[:, :, ti * 128:(ti + 1) * 128], hn2T_d[Bk * 8 + ti].rearrange("p (k t) -> p k t", k=8),
                          r=[bf("hn2T_d")], w=[bf("hn2Tb")])

            def final_tile(Bk, ti):
                i = Bk * 8 + ti
                s = i % 2
                P.dma("sp", xt[s][:], x1_d[i * 128:(i + 1) * 128, :], r=[bf("x1_d")], w=[Bxt[s]])
                if debug:
                    P.dma("sp", dbg["pe"][i * 128:(i + 1) * 128, :], acc[:, ti, :], r=[Bacc[ti * 2], Bacc[ti * 2 + 1]], w=[bf("dbg_pe")])
                P.tt("dve", x2[s][:], xt[s][:], acc[:, ti, :], ALU.add, r=[Bxt[s], Bacc[ti * 2], Bacc[ti * 2 + 1]], w=[Bx2[s]])
                P.act(junk[:], x2[s][:], AF.Square, r=[Bx2[s]], w=[bf("junk"), Bss[s]], accum_out=ss[s][:, 0:1])
                P.act(rs[s][:, 0:1], ss[s][:, 0:1], AF.Sqrt, r=[Bss[s]], w=[Bss[s]], scale=1.0 / D, bias=EPS)
                P.op("dve", lambda g, a=rs[s][:, 0:1]: g.reciprocal(out=a, in_=a), r=[Bss[s]], w=[Bss[s]])
                P.stt(ot[s][:], x2[s][:], rs[s][:, 0:1], gB[:], ALU.mult, ALU.mult, r=[Bx2[s], Bss[s], bf("gB")], w=[Bot[s]])
                P.dma("sp", out[i * 128:(i + 1) * 128, :], ot[s][:], r=[Bot[s]], w=[bf("out")])

            def load_group(Bk, gI):
                s = gI % 2
                P.dma("sp", Wblk[s][:], W_d[Bk * 16:(Bk + 1) * 16, :, gI * 8:(gI + 1) * 8, :].rearrange("h p c t -> p h c t"),
                      r=[bf("W_d")], w=[BW[s]])

            load_hn2Tb(0)
            load_group(0, 0)
            for Bk in range(4):
                for gI in range(16):
                    s = gI % 2
                    nb_, ng_ = (Bk, gI + 1) if gI < 15 else (Bk + 1, 0)
                    if nb_ < 4:
                        load_group(nb_, ng_)
                    for cc in range(8):
                        c = gI * 8 + cc
                        u = c % 3
                        P.dma("pool", Ub[u][:].rearrange("p k e -> p (k e)"), UT[c], w=[BU[u]])
                        P.dma("pool", Vb[s][:, cc, :], Vd[c * 128:(c + 1) * 128, :], w=[BV[s]])
                        for nb in range(2):
                            pa = (c % 2) * 2 + nb
                            for kc in range(8):
                                P.mm(ps[pa][:], Ub[u][:, kc, :], hn2Tb[:, kc, nb * 512:(nb + 1) * 512], kc == 0, kc == 7,
                                     r=[BU[u], bf("hn2Tb")], w=[Bps[pa]])
                            gi = (c * 2 + nb) % 3
                            P.act(gel[gi][:], ps[pa][:], GELU, r=[Bps[pa]], w=[Bgel[gi]])
                            P.tt("dve", Gg[s][:, cc, nb * 512:(nb + 1) * 512].rearrange("p (h t) -> p h t", h=8),
                                 gel[gi][:].rearrange("p (h t) -> p h t", h=8), Wblk[s][:, nb * 8:(nb + 1) * 8, cc, :], ALU.mult,
                                 r=[Bgel[gi], BW[s]], w=[BG[s]])
                    if gI == 15 and Bk < 3:
                        load_hn2Tb(Bk + 1)
                    for ti in range(8):
                        for half in range(2):
                            po = 4 + (ti * 2 + half) % 4
                            for cc in range(8):
                                P.mm(ps[po][:], Gg[s][:, cc, ti * 128:(ti + 1) * 128], Vb[s][:, cc, half * 512:(half + 1) * 512], cc == 0, cc == 7,
                                     r=[BG[s], BV[s]], w=[Bps[po]])
                            dst = acc[:, ti, half * 512:(half + 1) * 512]
                            ba = Bacc[ti * 2 + half]
                            if gI == 0:
                                P.cp("dve", dst, ps[po][:], r=[Bps[po]], w=[ba])
                            else:
                                P.tt("dve", dst, ps[po][:], dst, ALU.add, r=[Bps[po], ba], w=[ba])
                        if gI == 15:
                            final_tile(Bk, ti)

        finals = [bf("out"), bf("dbg_pe"), bf("W_d"), bf("dbg_W"), bf("x1_d"), bf("hn2T_d"), bf("mixA_d"), bf("sgB_d"), bf("dbg_y"), bf("dbg_ob"), bf("dbg_c")]
        P.emit(finals)
    return nc


def make_in_maps(inputs):
    f32 = lambda a: np.ascontiguousarray(np.asarray(a, dtype=np.float32))
    convw_l = f32(np.asarray(inputs["conv_w"]).reshape(31, 8, 128).transpose(2, 1, 0))
    cvec = np.stack([np.asarray(inputs[k]).reshape(8, 128).T for k in ("conv_b", "conv_ln_g", "conv_ln_b")], axis=1)
    shared = {
        "w_in": f32(inputs["w_in"]), "norm1_g": f32(inputs["norm1_g"]), "norm2_g": f32(inputs["norm2_g"]),
        "normf_g": f32(inputs["normf_g"]), "convw_l": convw_l, "cvec_l": f32(cvec), "fox_bf": f32(inputs["fox_bf"]),
        "w_conv_out": f32(inputs["w_conv_out"]), "w_fox_out": f32(inputs["w_fox_out"]), "w_out": f32(inputs["w_out"]),
    }
    wq = np.asarray(inputs["peer_wq"], dtype=np.float32)
    wqT = wq.T.reshape(8, 2, 128, 1024).transpose(1, 0, 2, 3).reshape(16, 128, 1024)
    kk = np.stack([np.asarray(inputs["peer_k1"]), np.asarray(inputs["peer_k2"])], axis=0)
    kT = kk.transpose(0, 1, 3, 2).reshape(16, 128, 128)
    U = np.asarray(inputs["peer_u"], dtype=np.float32)
    UT = U.reshape(128, 128, 8, 128).transpose(0, 3, 2, 1).reshape(128, 128, 1024)
    shared.update({"wqT_l": f32(wqT), "kT_l": f32(kT), "UT_l": f32(UT), "peer_v": f32(inputs["peer_v"])})
    x = np.asarray(inputs["x"], dtype=np.float32)
    return [dict(shared, x=np.ascontiguousarray(x[b])) for b in range(x.shape[0])]


def kernel(**inputs):
    nc = build()
    in_maps = make_in_maps(inputs)
    res = run_bass_kernel_spmd(nc, in_maps, core_ids=list(range(8)))
    return np.stack([r["out"] for r in res.results], axis=0)
```

```python
import os
from contextlib import ExitStack
import numpy as np
import concourse.bass as bass
import concourse.mybir as mybir
from concourse.bass_utils import run_bass_kernel_spmd

F32 = mybir.dt.float32
BF16 = mybir.dt.bfloat16
AF = mybir.ActivationFunctionType
ALU = mybir.AluOpType

S = 4096
D = 1024
NT = S // 128
EPS = 1e-6
PHASES = int(os.environ.get("MK_PHASES", "99"))


class Buf:
    __slots__ = ("lw", "rd")

    def __init__(self):
        self.lw = None
        self.rd = {}


class Arena:
    def __init__(self, ap, nelem):
        self.ap = ap
        self.n = nelem
        self.off = 0

    def alloc(self, shape, dtype=F32):
        size = 4 if dtype == F32 else 2
        nb = size * int(np.prod(shape[1:]))
        nel = (nb + 63) // 64 * 32
        assert self.off + nel <= self.n, ("SBUF arena overflow", self.off, nel, self.n)
        v = self.ap[:, self.off:self.off + nb // 2]
        self.off += nel
        if dtype == F32:
            v = v.bitcast(F32)
        if len(shape) == 3:
            v = v.rearrange("p (a b) -> p a b", a=shape[1])
        elif len(shape) == 4:
            v = v.rearrange("p (a b c) -> p a b c", a=shape[1], b=shape[2])
        return v

    def mark(self):
        return self.off

    def release(self, m):
        self.off = m


class Prog:
    def __init__(self, nc, es, ndma=40):
        self.nc = nc
        self.names = ["sp", "act", "dve", "pool", "pe"]
        self.sem = {k: es.enter_context(nc.semaphore("S_" + k)) for k in self.names}
        self.cnt = {k: 0 for k in self.names}
        self.stream = {k: [] for k in self.names}
        self.seen = {k: {} for k in self.names}
        self.dsem = [es.enter_context(nc.semaphore("D%d" % i)) for i in range(ndma)]
        self.dcnt = [0] * ndma
        self.dnext = 0

    def op(self, e, fn, r=(), w=(), dma=False):
        deps = []
        for b in r:
            if b.lw is not None:
                deps.append(b.lw)
        for b in w:
            if b.lw is not None:
                deps.append(b.lw)
            deps.extend(b.rd.values())
        waits = []
        seen = self.seen[e]
        for (s, v, se) in deps:
            if se == "pe" and e == "pe":
                continue
            if se == e and self.cnt[e] + 1 - v >= 4:
                continue
            key = id(s)
            if seen.get(key, 0) >= v:
                continue
            seen[key] = v
            waits.append((s, v))
        if dma:
            i = self.dnext
            self.dnext = (i + 1) % len(self.dsem)
            s = self.dsem[i]
            pv = self.dcnt[i]
            if pv > 0 and seen.get(id(s), 0) < pv:
                seen[id(s)] = pv
                waits.append((s, pv))
            self.dcnt[i] += 16
            tok = (s, self.dcnt[i], "dma")
            inc = (s, 16)
        else:
            self.cnt[e] += 1
            tok = (self.sem[e], self.cnt[e], e)
            inc = (self.sem[e], 1)
        self.stream[e].append((waits, fn, inc))
        for b in r:
            old = b.rd.get(id(tok[0]))
            if old is None or old[1] < tok[1]:
                b.rd[id(tok[0])] = tok
        for b in w:
            b.lw = tok
            b.rd = {}
        return tok

    def barrier(self):
        targets = [(self.sem[k], self.cnt[k]) for k in self.names if self.cnt[k] > 0]
        targets += [(self.dsem[i], self.dcnt[i]) for i in range(len(self.dsem)) if self.dcnt[i] > 0]
        for e in self.names:
            seen = self.seen[e]
            ws = []
            for s, v in targets:
                if seen.get(id(s), 0) < v:
                    seen[id(s)] = v
                    ws.append((s, v))
            if ws:
                self.stream[e].append((ws, None, None))

    def dma(self, e, out, in_, r=(), w=()):
        return self.op(e, lambda g: g.dma_start(out=out, in_=in_), r=r, w=w, dma=True)

    def mm(self, out, lhsT, rhs, start, stop, r=(), w=()):
        return self.op("pe", lambda g: g.matmul(out, lhsT, rhs, start=start, stop=stop), r=r, w=w)

    def tr(self, out, in_, ident, r=(), w=()):
        return self.op("pe", lambda g: g.transpose(out, in_, ident), r=r, w=w)

    def act(self, out, in_, func, r=(), w=(), e="act", **kw):
        return self.op(e, lambda g: g.activation(out=out, in_=in_, func=func, **kw), r=r, w=w)

    def tt(self, e, out, in0, in1, op, r=(), w=()):
        return self.op(e, lambda g: g.tensor_tensor(out=out, in0=in0, in1=in1, op=op), r=r, w=w)

    def ts(self, e, out, in0, s1, s2, op0, op1=None, r=(), w=()):
        if op1 is None:
            return self.op(e, lambda g: g.tensor_scalar(out=out, in0=in0, scalar1=s1, scalar2=None, op0=op0), r=r, w=w)
        return self.op(e, lambda g: g.tensor_scalar(out=out, in0=in0, scalar1=s1, scalar2=s2, op0=op0, op1=op1), r=r, w=w)

    def stt(self, out, in0, scalar, in1, op0, op1, r=(), w=()):
        return self.op("dve", lambda g: g.scalar_tensor_tensor(out=out, in0=in0, scalar=scalar, in1=in1, op0=op0, op1=op1), r=r, w=w)

    def cp(self, e, out, in_, r=(), w=()):
        if e == "act":
            return self.op(e, lambda g: g.copy(out=out, in_=in_), r=r, w=w)
        return self.op(e, lambda g: g.tensor_copy(out=out, in_=in_), r=r, w=w)

    def emit(self, final_bufs):
        waits = []
        for b in final_bufs:
            if b.lw is not None:
                waits.append((b.lw[0], b.lw[1]))
        nc = self.nc
        streams = self.stream
        with nc.Block() as block:
            def mk(name, extra):
                def body(g):
                    for ws, fn, inc in streams[name]:
                        for s, v in ws:
                            g.wait_ge(s, v)
                        if fn is not None:
                            fn(g).then_inc(inc[0], inc[1])
                    for s, v in extra:
                        g.wait_ge(s, v)
                return body
            block.sync(mk("sp", waits))
            block.scalar(mk("act", []))
            block.vector(mk("dve", []))
            block.gpsimd(mk("pool", []))
            block.tensor(mk("pe", []))


def build(debug=False):
    nc = bass.Bass("TRN2", target_bir_lowering=False)
    dt = lambda name, shape, dtype=F32, kind="ExternalInput": nc.dram_tensor(name, shape, dtype, kind=kind).ap()
    x = dt("x", [S, D])
    w_in = dt("w_in", [D, 7184])
    g1 = dt("norm1_g", [D])
    g2 = dt("norm2_g", [D])
    gf = dt("normf_g", [D])
    convw = dt("convw_l", [128, 8, 31])
    cvec = dt("cvec_l", [128, 3, 8])
    bfv = dt("fox_bf", [16])
    wco = dt("w_conv_out", [D, D])
    wfo = dt("w_fox_out", [D, D])
    wout = dt("w_out", [D, D])
    out = dt("out", [S, D], F32, "ExternalOutput")
    mixA_d = dt("mixA_d", [D, S], BF16, "Internal")
    sgB_d = dt("sgB_d", [D, S], BF16, "Internal")
    x1_d = dt("x1_d", [S, D], F32, "ExternalOutput" if debug else "Internal")
    hn2T_d = dt("hn2T_d", [NT, 128, 1024], BF16, "Internal")
    wqT = dt("wqT_l", [16, 128, 1024])
    kTd = dt("kT_l", [16, 128, 128])
    UT = dt("UT_l", [128, 128, 1024])
    Vd = dt("peer_v", [16384, 1024])
    W_d = dt("W_d", [64, 128, 128, 64], BF16, "Internal")
    CR_d = dt("CR_d", [16, 3, S], BF16, "Internal")
    WK_d = dt("WK_d", [128, 8 * 2048], BF16, "Internal")
    dbg = {}
    if debug:
        dbg["W"] = dt("dbg_W", [128, 128 * 64], BF16, "ExternalOutput")
        dbg["pe"] = dt("dbg_pe", [S, D], F32, "ExternalOutput")
        dbg["ob"] = dt("dbg_ob", [D, S], BF16, "ExternalOutput")
        dbg["y"] = dt("dbg_y", [D, S], BF16, "ExternalOutput")
        dbg["c"] = dt("dbg_c", [128, NT * 16], F32, "ExternalOutput")

    with ExitStack() as es:
        P = Prog(nc, es)
        NARENA = 105984
        arena_t = es.enter_context(nc.sbuf_tensor("arena", [128, NARENA], BF16))
        A = Arena(arena_t[:], NARENA)
        sb = lambda name, shape, dtype=F32: A.alloc(shape, dtype)
        ss = [sb("ss%d" % i, [128, 16]) for i in range(2)]
        rs = [sb("rs%d" % i, [128, 16]) for i in range(2)]
        gB = sb("gB", [128, D])
        identb = sb("identb", [128, 128], BF16)
        identf = sb("identf", [128, 128])
        onesb = sb("onesb", [128, 128], BF16)
        onesf = sb("onesf", [128, 128])
        trif = sb("trif", [128, 128])
        sel_e = sb("sel_e", [128, 128])
        sel_o = sb("sel_o", [128, 128])
        convw_s = sb("convw_s", [128, 8, 31])
        cvec_s = sb("cvec_s", [128, 3, 8])
        bfB = sb("bfB", [128, 16])
        PM0 = A.mark()
        RA = sb("RA", [128, 8, S], BF16)
        RB = sb("RB", [128, 8, S], BF16)
        PM = A.mark()

        ps = [es.enter_context(nc.psum_tensor("ps%d" % i, [128, 512], F32)) for i in range(8)]
        Bps = [Buf() for _ in range(8)]

        B = {}
        def bf(name):
            if name not in B:
                B[name] = Buf()
            return B[name]

        P.op("pool", lambda g: g.memset(onesf[:], 1.0), w=[bf("onesf")])
        P.op("pool", lambda g: g.memset(onesb[:], 1.0), w=[bf("onesb")])
        P.op("pool", lambda g: g.affine_select(out=identf[:], in_=onesf[:], pattern=[[-1, 128]], compare_op=ALU.is_equal,
                                              fill=0.0, base=0, channel_multiplier=1), r=[bf("onesf")], w=[bf("identf")])
        P.cp("pool", identb[:], identf[:], r=[bf("identf")], w=[bf("identb")])
        P.op("pool", lambda g: g.affine_select(out=trif[:], in_=onesf[:], pattern=[[1, 128]], compare_op=ALU.is_ge,
                                              fill=0.0, base=0, channel_multiplier=-1), r=[bf("onesf")], w=[bf("trif")])
        P.op("pool", lambda g: g.affine_select(out=sel_e[:], in_=onesf[:], pattern=[[0, 128]], compare_op=ALU.is_equal,
                                              fill=0.0, base=-64, channel_multiplier=1), r=[bf("onesf")], w=[bf("sel_e")])
        P.op("pool", lambda g: g.affine_select(out=sel_o[:], in_=onesf[:], pattern=[[0, 128]], compare_op=ALU.is_equal,
                                              fill=0.0, base=0, channel_multiplier=1), r=[bf("onesf")], w=[bf("sel_o")])
        P.dma("sp", gB[:], g1.partition_broadcast(128), w=[bf("gB")])
        P.dma("sp", convw_s[:], convw, w=[bf("convw")])
        P.dma("sp", cvec_s[:], cvec, w=[bf("cvec")])
        P.dma("sp", bfB[:], bfv.partition_broadcast(128), w=[bf("bfB")])

        hnT = RA
        Bxt = [Buf(), Buf()]
        Bhn = [Buf(), Buf()]
        Bss = [Buf(), Buf()]

        def rmsnorm_to_T(src_tile, srcbuf, s, i, dstT, dstbuf, psb, gtile, gbuf):
            P.act(junk[:], src_tile[:], AF.Square, r=[srcbuf], w=[bf("junk"), Bss[s]], accum_out=ss[s][:, 0:1])
            P.act(rs[s][:, 0:1], ss[s][:, 0:1], AF.Sqrt, r=[Bss[s]], w=[Bss[s]], scale=1.0 / D, bias=EPS)
            P.op("dve", lambda g: g.reciprocal(out=rs[s][:, 0:1], in_=rs[s][:, 0:1]), r=[Bss[s]], w=[Bss[s]])
            P.stt(hn[s][:], src_tile[:], rs[s][:, 0:1], gtile[:], ALU.mult, ALU.mult, r=[srcbuf, Bss[s], gbuf], w=[Bhn[s]])
            pv = ps[psb][:].bitcast(BF16)
            for k in range(8):
                P.tr(pv[:, k * 128:(k + 1) * 128], hn[s][:, k * 128:(k + 1) * 128], identb[:],
                     r=[Bhn[s], bf("identb")], w=[Bps[psb]])
            return pv

        xt = [sb("xt%d" % i, [128, D]) for i in range(2)]
        hn = [sb("hn%d" % i, [128, D], BF16) for i in range(2)]
        junk = sb("junk", [128, D], BF16)
        RBf = RB.rearrange("p k t -> p (k t)")
        WKv = RBf[:, 0:16384].rearrange("p (k n) -> p k n", k=8)
        wqs = [RBf[:, 16384 + i * 1024:16384 + (i + 1) * 1024] for i in range(2)]
        kts = [RBf[:, 18432 + i * 128:18432 + (i + 1) * 128] for i in range(2)]
        Bwq = [Buf(), Buf()]

        def wk_group(g_):
            s = g_ % 2
            P.dma("pool", wqs[s], wqT[g_], w=[Bwq[s]])
            P.dma("pool", kts[s], kTd[g_], w=[Bwq[s]])
            for kc in range(8):
                pb = 2 + (g_ * 8 + kc) // 4 % 2
                sl = (g_ * 8 + kc) % 4
                P.mm(ps[pb][:, sl * 128:(sl + 1) * 128], wqs[s][:, kc * 128:(kc + 1) * 128], kts[s], True, True,
                     r=[Bwq[s]], w=[Bps[pb]])
                if sl == 3:
                    kc0 = kc - 3
                    P.cp("dve", WKv[:, kc0:kc0 + 4, g_ * 128:(g_ + 1) * 128],
                         ps[pb][:].rearrange("p (k e) -> p k e", k=4), r=[Bps[pb]], w=[bf("WKv")])

        for i in range(NT):
            s = i % 2
            P.dma("sp", xt[s][:], x[i * 128:(i + 1) * 128, :], w=[Bxt[s]])
            pv = rmsnorm_to_T(xt[s], Bxt[s], s, i, hnT, bf("RA"), s, gB, bf("gB"))
            P.cp("act", hnT[:, :, i * 128:(i + 1) * 128], pv.rearrange("p (k t) -> p k t", k=8),
                 r=[Bps[s]], w=[bf("RA")])
            if PHASES >= 8 and i % 2 == 1:
                wk_group(i // 2)
        if PHASES >= 8:
            P.dma("sp", WK_d, WKv.rearrange("p k n -> p (k n)"), r=[bf("WKv")], w=[bf("WK_d")])

        P.barrier()
        A.release(PM)

        def load_w(dst, dbuf, src2d, col0, ncol=128):
            return P.dma("pool", dst[:, :, 0:ncol], src2d[:, col0:col0 + ncol].rearrange("(k p) j -> p k j", p=128), w=[dbuf])

        BwA = [Buf(), Buf()]
        BwB = [Buf(), Buf()]
        BwC = [Buf(), Buf()]
        Btb = [Buf() for _ in range(3)]
        Btf = [Buf() for _ in range(4)]
        yT = RB
        BY = [Buf() for _ in range(8)]
        wA = [sb("wA%d" % i, [128, 8, 128], BF16) for i in range(2)]
        wBt = [sb("wB%d" % i, [128, 8, 128], BF16) for i in range(2)]
        wC = [sb("wC%d" % i, [128, 8, 128], BF16) for i in range(2)]
        PMW = A.mark()
        tmpb = [sb("tmpb%d" % i, [128, 512], BF16) for i in range(3)]
        tmpf = [sb("tmpf%d" % i, [128, 512]) for i in range(4)]
        big = [sb("big%d" % i, [128, 4128], BF16) for i in range(2)]
        PM2 = A.mark()

        def proj(psb, wt, wbuf, src, srcbuf, tb):
            for kc in range(8):
                sbf = srcbuf[kc] if isinstance(srcbuf, list) else srcbuf
                P.mm(ps[psb][:, :], wt[:, kc, :], src[:, kc, tb * 512:(tb + 1) * 512], kc == 0, kc == 7,
                     r=[wbuf, sbf], w=[Bps[psb]])

        if PHASES >= 2:
            dgs = [sb("dg%d" % i, [128, 31, 128], BF16) for i in range(2)]
            Bdg = [Buf(), Buf()]
            for i in range(2):
                P.op("dve", lambda g, ap=big[i][:, 0:32]: g.memset(ap, 0.0), w=[bf("big%d" % i)])
            Bzc = [[Buf() for _ in range(9)] for _ in range(2)]
            it = 0

            def p2_prefetch(c):
                s = c % 2
                load_w(wA[s], BwA[s], w_in, c * 128)
                load_w(wBt[s], BwB[s], w_in, 1024 + c * 128)
                P.tt("pool", dgs[s][:], identf[:].unsqueeze(1).to_broadcast([128, 31, 128]),
                     convw_s[:, c, :].unsqueeze(2).to_broadcast([128, 31, 128]), ALU.mult,
                     r=[bf("identf"), bf("convw")], w=[Bdg[s]])

            p2_prefetch(0)
            for c in range(8):
                s = c % 2
                if c + 1 < 8:
                    p2_prefetch(c + 1)
                zc = big[s]
                dg = dgs[s]

                def p2_proj(tb):
                    pa, pb = 0 + (tb % 2) * 3, 1 + (tb % 2) * 3
                    proj(pa, wA[s], BwA[s], hnT, bf("RA"), tb)
                    proj(pb, wBt[s], BwB[s], hnT, bf("RA"), tb)
                    tbi = (c * 8 + tb) % 3
                    P.act(tmpb[tbi][:], ps[pb][:], AF.Sigmoid, r=[Bps[pb]], w=[Btb[tbi]])
                    P.tt("dve", zc[:, 30 + tb * 512: 30 + (tb + 1) * 512], ps[pa][:], tmpb[tbi][:], ALU.mult,
                         r=[Bps[pa], Btb[tbi], bf("big%d" % s)], w=[Bzc[s][tb + 1]])

                p2_proj(0)
                for tb in range(8):
                    if tb + 1 < 8:
                        p2_proj(tb + 1)
                    py = 2 + (tb % 2) * 3
                    for w_ in range(31):
                        P.mm(ps[py][:], dg[:, w_, :], zc[:, tb * 512 + w_: tb * 512 + w_ + 512], w_ == 0, w_ == 30,
                             r=[Bdg[s], Bzc[s][tb], Bzc[s][tb + 1], bf("big%d" % s)], w=[Bps[py]])
                    P.act(yT[:, c, tb * 512:(tb + 1) * 512], ps[py][:], AF.Identity, r=[Bps[py], bf("cvec")], w=[BY[c]],
                          bias=cvec_s[:, 0, c:c + 1])
            if debug:
                for c in range(8):
                    P.dma("sp", dbg["y"][c * 128:(c + 1) * 128, :], yT[:, c, :], r=[BY[c]], w=[bf("dbg_y")])

        P.barrier()
        A.release(PM2)
        if PHASES >= 3:
            ub = [sb("ub%d" % i, [128, 512]) for i in range(3)]
            u2 = [sb("u2%d" % i, [128, 512]) for i in range(3)]
            Bub = [Buf() for _ in range(3)]
            Bu2 = [Buf() for _ in range(3)]
            for tb in range(8):
                blk = slice(tb * 512, (tb + 1) * 512)
                for c in range(8):
                    tbi = c % 3
                    P.act(tmpb[tbi][:], yT[:, c, blk], AF.Square, r=[BY[c]], w=[Btb[tbi]])
                    P.mm(ps[0][:], onesb[:], yT[:, c, blk], c == 0, c == 7, r=[bf("onesb"), BY[c]], w=[Bps[0]])
                    P.mm(ps[1][:], onesb[:], tmpb[tbi][:], c == 0, c == 7, r=[bf("onesb"), Btb[tbi]], w=[Bps[1]])
                mean, msq, var, nmr = tmpf[0], tmpf[1], tmpf[2], tmpf[3]
                P.ts("dve", mean[:], ps[0][:], 1.0 / 1024, None, ALU.mult, r=[Bps[0]], w=[Btf[0]])
                P.tt("dve", msq[:], mean[:], mean[:], ALU.mult, r=[Btf[0]], w=[Btf[1]])
                P.stt(var[:], ps[1][:], 1.0 / 1024, msq[:], ALU.mult, ALU.subtract, r=[Bps[1], Btf[1]], w=[Btf[2]])
                P.act(var[:], var[:], AF.Sqrt, r=[Btf[2]], w=[Btf[2]], bias=EPS, scale=1.0)
                P.op("dve", lambda g, var=var: g.reciprocal(out=var[:], in_=var[:]), r=[Btf[2]], w=[Btf[2]])
                P.stt(nmr[:], mean[:], -1.0, var[:], ALU.mult, ALU.mult, r=[Btf[0], Btf[2]], w=[Btf[3]])
                for c in range(8):
                    k = c % 3
                    P.tt("dve", ub[k][:], yT[:, c, blk], var[:], ALU.mult, r=[BY[c], Btf[2]], w=[Bub[k]])
                    P.tt("pool", u2[k][:], ub[k][:], nmr[:], ALU.add, r=[Bub[k], Btf[3]], w=[Bu2[k]])
                    P.act(yT[:, c, blk], u2[k][:], AF.Silu, r=[Bu2[k], bf("cvec")], w=[BY[c]],
                          scale=cvec_s[:, 1, c:c + 1], bias=cvec_s[:, 2, c:c + 1])

        if PHASES >= 4:
            it = 0
            for n in range(8):
                s = n % 2
                load_w(wA[s], BwA[s], wco, n * 128)
                load_w(wBt[s], BwB[s], w_in, 5136 + n * 128)
                load_w(wC[s], BwC[s], w_in, 6160 + n * 128)
                for tb in range(8):
                    blk = slice(tb * 512, (tb + 1) * 512)
                    o = (tb % 2) * 3
                    proj(o + 0, wA[s], BwA[s], yT, BY, tb)
                    proj(o + 1, wBt[s], BwB[s], hnT, bf("RA"), tb)
                    proj(o + 2, wC[s], BwC[s], hnT, bf("RA"), tb)
                    tbi = it % 3
                    it += 1
                    P.act(tmpb[tbi][:], ps[o + 1][:], AF.Sigmoid, r=[Bps[o + 1]], w=[Btb[tbi]])
                    P.tt("dve", big[0][:, blk], ps[o + 0][:], tmpb[tbi][:], ALU.mult, r=[Bps[o + 0], Btb[tbi]], w=[bf("big0")])
                    P.act(big[1][:, blk], ps[o + 2][:], AF.Sigmoid, r=[Bps[o + 2]], w=[bf("big1")])
                P.dma("sp", mixA_d[n * 128:(n + 1) * 128, :], big[0][:, 0:S], r=[bf("big0")], w=[bf("mixA_d")])
                P.dma("sp", sgB_d[n * 128:(n + 1) * 128, :], big[1][:, 0:S], r=[bf("big1")], w=[bf("sgB_d")])

        P.barrier()
        A.release(PMW)
        if PHASES >= 5:
            fb = sb("fb", [128, NT, 16])
            Cc = sb("Cc", [128, NT, 16])
            crefB = sb("crefB", [128, 16, 16])
            qT = [sb("qT%d" % i, [128, S], BF16) for i in range(2)]
            kTa = [sb("kT%d" % i, [128, S], BF16) for i in range(2)]
            cres = [fb, Cc]
            Vaug = sb("Vaug", [128, NT, 193], BF16)
            Rz_e = sb("Rz_e", [128, 256])
            Rz_o = sb("Rz_o", [128, 256])
            rb = [sb("rb%d" % i, [128, 256]) for i in range(2)]
            PM5 = A.mark()
            wf_s = sb("wf_s", [128, 8, 16], BF16)
            incl = fb
            raw12 = sb("raw12", [128, 2 * NT * 16], BF16)
            Tsb = raw12.bitcast(F32).rearrange("p (i h) -> p i h", h=16)
            csp = [sb("csp0", [128, NT, 16], BF16), raw12[:, 0:NT * 16].rearrange("p (i h) -> p i h", h=16),
                   raw12[:, NT * 16:2 * NT * 16].rearrange("p (i h) -> p i h", h=16)]
            cstg1 = sb("cstg", [16, 384], BF16)
            cstg = [cstg1, cstg1]
            P.op("pool", lambda g: g.memset(Rz_e[:], 0.0), w=[bf("Rz_e")])
            P.op("pool", lambda g: g.memset(Rz_o[:], 0.0), w=[bf("Rz_o")])
            P.op("pool", lambda g: g.memset(qT[0][64:128, :], 0.0), w=[bf("qT")])
            P.op("pool", lambda g: g.memset(qT[1][0:64, :], 0.0), w=[bf("qT")])
            P.op("pool", lambda g: g.memset(qT[0][64:68, :], 8.0), w=[bf("qT")])
            P.op("pool", lambda g: g.memset(qT[1][0:4, :], 8.0), w=[bf("qT")])
            P.op("pool", lambda g: g.memset(kTa[0][64:128, :], 0.0), w=[bf("kT")])
            P.op("pool", lambda g: g.memset(kTa[1][0:64, :], 0.0), w=[bf("kT")])
            P.op("pool", lambda g: g.memset(kTa[0][64:65, :], 1.0), w=[bf("kT")])
            P.op("pool", lambda g: g.memset(kTa[1][0:1, :], 1.0), w=[bf("kT")])
            P.op("pool", lambda g: g.memset(Vaug[:], 0.0), w=[bf("Vaug")])
            P.op("pool", lambda g: g.memset(Vaug[:, :, 64:66], 1.0), w=[bf("Vaug")])
            P.dma("pool", wf_s[:], w_in[:, 5120:5136].rearrange("(k p) j -> p k j", p=128), w=[bf("wf")])
            for i in range(NT):
                for kc in range(8):
                    P.mm(ps[0][:, i * 16:(i + 1) * 16], hnT[:, kc, i * 128:(i + 1) * 128], wf_s[:, kc, :], kc == 0, kc == 7,
                         r=[bf("RA"), bf("wf")], w=[Bps[0]])
            P.tt("dve", fb[:], ps[0][:].rearrange("p (i h) -> p i h", h=16), bfB[:].unsqueeze(1).to_broadcast([128, NT, 16]),
                 ALU.add, r=[Bps[0], bf("bfB")], w=[bf("fb")])
            P.act(fb[:], fb[:], AF.Exp, r=[bf("fb")], w=[bf("fb")], scale=-1.0)
            P.act(fb[:], fb[:], AF.Ln, r=[bf("fb")], w=[bf("fb")], bias=1.0, scale=1.0)
            P.ts("dve", fb[:], fb[:], -1.0, None, ALU.mult, r=[bf("fb")], w=[bf("fb")])
            for i in range(NT):
                P.mm(ps[1][:, i * 16:(i + 1) * 16], trif[:], fb[:, i, :], True, True, r=[bf("trif"), bf("fb")], w=[Bps[1]])
            for i in range(NT):
                P.mm(ps[2][:, i * 16:(i + 1) * 16], onesf[:], fb[:, i, :], True, True, r=[bf("onesf"), bf("fb")], w=[Bps[2]])
            P.cp("dve", Tsb[:], ps[2][:].rearrange("p (i h) -> p i h", h=16), r=[Bps[2]], w=[bf("csp")])
            for h in range(16):
                P.op("dve", lambda g, o=incl[:, :, h], d0=onesf[:, 0:NT], d1=Tsb[:, :, h]: g.tensor_tensor_scan(
                    out=o, data0=d0, data1=d1, initial=0.0, op0=ALU.mult, op1=ALU.add),
                    r=[bf("csp"), bf("onesf")], w=[bf("fb")])
            P.cp("dve", crefB[:], incl[:].rearrange("p (q two) h -> p q two h", two=2)[:, :, 0, :], r=[bf("fb")], w=[bf("crefB")])
            P.tt("dve", Tsb[:], incl[:], Tsb[:], ALU.subtract, r=[bf("fb"), bf("csp")], w=[bf("csp")])
            P.tt("dve", Cc[:], ps[1][:].rearrange("p (i h) -> p i h", h=16), Tsb[:], ALU.add, r=[Bps[1], bf("csp")], w=[bf("Cc")])
            if debug:
                P.dma("sp", dbg["c"], Cc[:].rearrange("p i h -> p (i h)"), r=[bf("Cc")], w=[bf("dbg_c")])
            P.ts("dve", cres[0][:], Cc[:], -1.0, None, ALU.mult, r=[bf("Cc")], w=[bf("fb")])
            P.cp("dve", csp[0][:], cres[0][:], r=[bf("fb")], w=[bf("csp")])
            P.tt("dve", cres[1][:], cres[0][:], csp[0][:], ALU.subtract, r=[bf("fb"), bf("csp")], w=[bf("Cc")])
            P.cp("dve", csp[1][:], cres[1][:], r=[bf("Cc")], w=[bf("csp")])
            P.tt("dve", cres[0][:], cres[1][:], csp[1][:], ALU.subtract, r=[bf("Cc"), bf("csp")], w=[bf("fb")])
            P.cp("dve", csp[2][:], cres[0][:], r=[bf("fb")], w=[bf("csp")])
            Bc1 = Buf()
            Bcst = [Bc1, Bc1]
            for i in range(NT):
                k = i % 2
                pvc = ps[3 + k][:].bitcast(BF16)
                for r_ in range(3):
                    P.tr(pvc[0:16, r_ * 128:(r_ + 1) * 128], csp[r_][:, i, :], identb[:], r=[bf("csp"), bf("identb")], w=[Bps[3 + k]])
                P.cp("act", cstg[k][0:16, :], pvc[0:16, 0:384], r=[Bps[3 + k]], w=[Bcst[k]])
                P.dma("sp", CR_d[:, :, i * 128:(i + 1) * 128], cstg[k][0:16, :].rearrange("p (r t) -> p r t", r=3), r=[Bcst[k]], w=[bf("CR_d")])

            P.barrier()
            A.release(PM5)
            pT = [sb("pT%d" % i, [128, 512], BF16) for i in range(4)]
            obT = RB
            BpT = [Buf() for _ in range(4)]
            Brb = [Buf(), Buf()]
            ipt = 0
            def p5_loads(hp):
                s = hp % 2
                load_w(wA[s], BwA[s], w_in, 2048 + hp * 128)
                load_w(wBt[s], BwB[s], w_in, 3072 + hp * 128)
                load_w(wC[s], BwC[s], w_in, 4096 + hp * 128)

            p5_loads(0)
            for hp in range(8):
                s = hp % 2
                P.dma("sp", kTa[0][65:68, :], CR_d[2 * hp], r=[bf("CR_d")], w=[bf("kT")])
                P.dma("sp", kTa[1][1:4, :], CR_d[2 * hp + 1], r=[bf("CR_d")], w=[bf("kT")])
                P.ts("dve", qT[0][64:65, :].rearrange("p (q c) -> p q c", c=256),
                     crefB[64:65, :, 2 * hp:2 * hp + 1].to_broadcast([1, 16, 256]), 8.0, None, ALU.mult, r=[bf("crefB")], w=[bf("qT")])
                P.ts("dve", qT[1][0:1, :].rearrange("p (q c) -> p q c", c=256),
                     crefB[0:1, :, 2 * hp + 1:2 * hp + 2].to_broadcast([1, 16, 256]), 8.0, None, ALU.mult, r=[bf("crefB")], w=[bf("qT")])
                for tb in range(8):
                    blk = slice(tb * 512, (tb + 1) * 512)
                    proj(0 + tb % 2, wA[s], BwA[s], hnT, bf("RA"), tb)
                    P.cp("act", qT[0][0:64, blk], ps[0 + tb % 2][0:64, :], r=[Bps[0 + tb % 2]], w=[bf("qT")])
                    P.cp("pool" if False else "dve", qT[1][64:128, blk], ps[0 + tb % 2][64:128, :], r=[Bps[0 + tb % 2]], w=[bf("qT")])
                    proj(2 + tb % 2, wBt[s], BwB[s], hnT, bf("RA"), tb)
                    P.cp("dve", kTa[0][0:64, blk], ps[2 + tb % 2][0:64, :], r=[Bps[2 + tb % 2]], w=[bf("kT")])
                    P.cp("dve", kTa[1][64:128, blk], ps[2 + tb % 2][64:128, :], r=[Bps[2 + tb % 2]], w=[bf("kT")])
                for i4 in range(8):
                    pb = 4 + i4 % 2
                    for ii in range(4):
                        i = i4 * 4 + ii
                        for kc in range(8):
                            P.mm(ps[pb][:, ii * 128:(ii + 1) * 128], hnT[:, kc, i * 128:(i + 1) * 128], wC[s][:, kc, :], kc == 0, kc == 7,
                                 r=[bf("RA"), BwC[s]], w=[Bps[pb]])
                    pv3 = ps[pb][:].rearrange("p (i c) -> p i c", c=128)
                    P.cp("act", Vaug[:, i4 * 4:(i4 + 1) * 4, 0:64], pv3[:, :, 0:64], r=[Bps[pb]], w=[bf("Vaug")])
                    P.cp("dve", Vaug[:, i4 * 4:(i4 + 1) * 4, 129:193], pv3[:, :, 64:128], r=[Bps[pb]], w=[bf("Vaug")])
                if hp + 1 < 8:
                    p5_loads(hp + 1)
                items = [(e_, Q, kt) for e_ in range(2) for Q in range(8) for kt in range(4 * Q + 4)]
                LA = 3

                def geom(Q, kt):
                    c0 = max(0, kt - 4 * Q) * 128
                    return c0, 512 - c0

                def s_exp(n):
                    e_, Q, kt = items[n]
                    c0, ncol = geom(Q, kt)
                    q0 = Q * 512 + c0
                    psb = n % 4
                    P.mm(ps[psb][:, 0:ncol], kTa[e_][:, kt * 128:(kt + 1) * 128], qT[e_][:, q0:q0 + ncol],
                         True, True, r=[bf("kT"), bf("qT")], w=[Bps[psb]])
                    P.act(pT[psb][:, 0:ncol], ps[psb][:, 0:ncol], AF.Exp, r=[Bps[psb]], w=[BpT[psb]], scale=0.125)
                    if kt >= 4 * Q:
                        P.op("pool", lambda g, a=pT[psb][:, 0:128]: g.affine_select(out=a, in_=a, pattern=[[1, 128]],
                                                                                     compare_op=ALU.is_ge, fill=0.0, base=0, channel_multiplier=-1),
                             r=[BpT[psb]], w=[BpT[psb]])

                def pv(n):
                    e_, Q, kt = items[n]
                    c0, ncol = geom(Q, kt)
                    po = 6 + Q % 2
                    nk = 4 * Q + 4
                    pi = n % 4
                    if e_ == 0:
                        P.mm(ps[po][0:65, c0:c0 + ncol], Vaug[:, kt, 0:65], pT[pi][:, 0:ncol], kt == 0, kt == nk - 1,
                             r=[bf("Vaug"), BpT[pi]], w=[Bps[po]])
                    else:
                        P.mm(ps[po][:, c0:c0 + ncol], Vaug[:, kt, 65:193], pT[pi][:, 0:ncol], kt == 0, kt == nk - 1,
                             r=[bf("Vaug"), BpT[pi]], w=[Bps[po]])
                    return kt == nk - 1

                def fin_a(e_, Q, half):
                    po = 6 + Q % 2
                    Rz, rrow = (Rz_e, 64) if e_ == 0 else (Rz_o, 0)
                    Bz = bf("Rz_e") if e_ == 0 else bf("Rz_o")
                    cs = slice(half * 256, (half + 1) * 256)
                    P.op("dve", lambda g, o=Rz[rrow:rrow + 1, :], a=ps[po][rrow:rrow + 1, cs]: g.reciprocal(out=o, in_=a),
                         r=[Bps[po]], w=[Bz])

                def fin_b(e_, Q, half):
                    po = 6 + Q % 2
                    pbase = 64 * e_
                    Rz, sel = (Rz_e, sel_e) if e_ == 0 else (Rz_o, sel_o)
                    Bz = bf("Rz_e") if e_ == 0 else bf("Rz_o")
                    pbc = 4 + half
                    cs = slice(half * 256, (half + 1) * 256)
                    P.mm(ps[pbc][:, 0:256], sel[:], Rz[:], True, True, r=[bf("sel_e"), bf("sel_o"), Bz], w=[Bps[pbc]])
                    ri = half
                    P.cp("dve", rb[ri][:], ps[pbc][:, 0:256], r=[Bps[pbc]], w=[Brb[ri]])
                    P.tt("dve", obT[pbase:pbase + 64, hp, Q * 512 + half * 256:Q * 512 + (half + 1) * 256], ps[po][pbase:pbase + 64, cs],
                         rb[ri][pbase:pbase + 64, :], ALU.mult, r=[Bps[po], Brb[ri]], w=[bf("RB")])

                pend = []
                for n in range(len(items) + LA):
                    if n < len(items):
                        s_exp(n)
                    for pe_ in pend:
                        pe_[0] -= 1
                    while pend and pend[0][0] <= 0:
                        _, e2_, q2_, h2_ = pend.pop(0)
                        fin_b(e2_, q2_, h2_)
                        if h2_ == 0:
                            fin_a(e2_, q2_, 1)
                            pend.insert(0, [6, e2_, q2_, 1])
                    if n >= LA:
                        if pv(n - LA):
                            while pend:
                                _, e3_, q3_, h3_ = pend.pop(0)
                                fin_b(e3_, q3_, h3_)
                                if h3_ == 0:
                                    fin_a(e3_, q3_, 1)
                                    fin_b(e3_, q3_, 1)
                            e2_, q2_, _ = items[n - LA]
                            fin_a(e2_, q2_, 0)
                            pend.append([6, e2_, q2_, 0])
                while pend:
                    _, e3_, q3_, h3_ = pend.pop(0)
                    fin_b(e3_, q3_, h3_)
                    if h3_ == 0:
                        fin_a(e3_, q3_, 1)
                        fin_b(e3_, q3_, 1)
            if debug:
                for c in range(8):
                    P.dma("sp", dbg["ob"][c * 128:(c + 1) * 128, :], obT[:, c, :], r=[bf("RB")], w=[bf("dbg_ob")])

        P.barrier()
        A.release(PMW)
        if PHASES >= 6:
            tmpf = [sb("tmpf%d" % i, [128, 512]) for i in range(4)]
            big = [sb("big%d" % i, [128, 4128], BF16) for i in range(2)]
            mixinT = RA
            for n in range(8):
                s = n % 2
                load_w(wA[s], BwA[s], wfo, n * 128)
                P.dma("sp", big[0][:, 0:S], mixA_d[n * 128:(n + 1) * 128, :], r=[bf("mixA_d")], w=[bf("big0")])
                P.dma("sp", big[1][:, 0:S], sgB_d[n * 128:(n + 1) * 128, :], r=[bf("sgB_d")], w=[bf("big1")])
                for tb in range(8):
                    blk = slice(tb * 512, (tb + 1) * 512)
                    pb = tb % 2
                    proj(pb, wA[s], BwA[s], obT, bf("RB"), tb)
                    ti = tb % 2
                    P.tt("dve", tmpf[ti][:], ps[pb][:], big[1][:, blk], ALU.mult, r=[Bps[pb], bf("big1")], w=[Btf[ti]])
                    P.tt("pool" if tb % 2 == 0 else "dve", mixinT[:, n, blk], tmpf[ti][:], big[0][:, blk], ALU.add,
                         r=[Btf[ti], bf("big0")], w=[bf("RA")])

            P.barrier()
            A.release(PM)
            woutS = sb("woutS", [128, 8, D], BF16)
            hn = [sb("hn%d" % i, [128, D], BF16) for i in range(2)]
            junk = sb("junk", [128, D], BF16)
            xt = [sb("xt%d" % i, [128, D]) for i in range(2)]
            x1t = [sb("x1t%d" % i, [128, D]) for i in range(2)]
            hst = [sb("hst%d" % i, [128, 1024], BF16) for i in range(2)]
            P.dma("pool", woutS[:], wout.rearrange("(k p) n -> p k n", p=128), w=[bf("wout")])
            P.dma("sp", gB[:], g2.partition_broadcast(128), r=[bf("gB")], w=[bf("gB")])
            Bx1 = [Buf(), Buf()]
            Bhst = [Buf(), Buf()]
            def p7_mm(i):
                s = i % 2
                P.dma("sp", xt[s][:], x[i * 128:(i + 1) * 128, :], w=[Bxt[s]])
                for half in range(2):
                    pb = 2 * s + half
                    for kc in range(8):
                        P.mm(ps[pb][:], mixinT[:, kc, i * 128:(i + 1) * 128], woutS[:, kc, half * 512:(half + 1) * 512], kc == 0, kc == 7,
                             r=[bf("RA"), bf("wout")], w=[Bps[pb]])
                    P.tt("dve", x1t[s][:, half * 512:(half + 1) * 512], ps[pb][:], xt[s][:, half * 512:(half + 1) * 512], ALU.add,
                         r=[Bps[pb], Bxt[s]], w=[Bx1[s]])
                P.dma("sp", x1_d[i * 128:(i + 1) * 128, :], x1t[s][:], r=[Bx1[s]], w=[bf("x1_d")])

            def p7_norm(i):
                s = i % 2
                pv = rmsnorm_to_T(x1t[s], Bx1[s], s, i, None, None, 4 + s, gB, bf("gB"))
                P.cp("act", hst[s][:], pv, r=[Bps[4 + s]], w=[Bhst[s]])
                P.dma("sp", hn2T_d[i], hst[s][:], r=[Bhst[s]], w=[bf("hn2T_d")])

            p7_mm(0)
            for i in range(NT):
                if i + 1 < NT:
                    p7_mm(i + 1)
                p7_norm(i)

        NTS = int(os.environ.get("MK_NTS", str(NT)))
        if PHASES >= 8:
            P.barrier()
            A.release(PM0)
            WK = sb("WK", [128, 8, 2048], BF16)
            P.dma("sp", WK[:].rearrange("p k n -> p (k n)"), WK_d, r=[bf("WK_d")], w=[bf("WK")])
            AX = mybir.AxisListType
            hst1 = sb("hst", [128, 1024], BF16)
            hst = [hst1, hst1]
            Ssb1 = sb("Ssb", [128, 16, 128])
            Ssb = [Ssb1, Ssb1]
            Vt = [sb("Vt%d" % i, [128, 16, 16]) for i in range(2)]
            SC = [sb("SC%d" % i, [128, 8, 16]) for i in range(2)]
            negm = [sb("negm%d" % i, [128, 8]) for i in range(2)]
            Zs = [sb("Zs%d" % i, [128, 8]) for i in range(2)]
            nbias = [sb("nbias%d" % i, [128, 8]) for i in range(2)]
            coefE = [sb("coefE%d" % i, [128, 8]) for i in range(2)]
            ed = sb("ed", [128, 8, 16])
            Stmp = sb("Stmp", [128, 128])
            cand = sb("cand", [128, 8, 256])
            candt = sb("candt", [128, 256])
            m1 = sb("m1", [128, 4, 128])
            S1m = m1
            sig = sb("sig", [128, 4, 7, 128])
            Es = sb("Es", [128, 4, 7, 128], BF16)

            def dual(n_bf16, pat, **kw):
                raw = sb("raw", [128, n_bf16], BF16)
                return raw.rearrange(pat[0], **kw), raw.bitcast(F32).rearrange(pat[1], **kw)
            A1d = [dual(8 * 7 * 128, ("p (h r e) -> p h r e", "p (h r e) -> p h r e"), h=8, r=7) for _ in range(2)]
            A2d = [dual(8 * 7 * 128, ("p (h r e) -> p h r e", "p (h r e) -> p h r e"), h=8, r=7) for _ in range(2)]
            A1T = sb("A1T", [128, 64, 64, 2], BF16)
            A2T = sb("A2T", [128, 128, 64], BF16)
            A2Tq = A2T.rearrange("p (q r) t -> p q r t", r=2)
            WTs2 = [sb("WTs%d" % i, [128, 128, 64], BF16) for i in range(2)]
            BWT = [Buf(), Buf()]
            Bh1 = Buf()
            Bhst = [Bh1, Bh1]
            BS1 = Buf()
            BS = [BS1, BS1]
            BV = [Buf(), Buf()]
            BSC = [Buf(), Buf()]
            Bst = [Buf(), Buf()]
            BA1 = [Buf(), Buf()]
            BA2 = [Buf(), Buf()]
            NEG = -1.0e30
            NJ = 56

            def xa_s(i):
                s = i % 2
                P.dma("sp", hst[s][:], hn2T_d[i], r=[bf("hn2T_d")], w=[Bhst[s]])
                for q4 in range(4):
                    for kc in range(8):
                        P.mm(ps[q4][:], hst[s][:, kc * 128:(kc + 1) * 128], WK[:, kc, q4 * 512:(q4 + 1) * 512], kc == 0, kc == 7,
                             r=[Bhst[s], bf("WK")], w=[Bps[q4]])
                    P.cp("dve", Ssb[s][:, q4 * 4:(q4 + 1) * 4, :], ps[q4][:].rearrange("p (g e) -> p g e", g=4),
                         r=[Bps[q4]], w=[BS[s]])

            def xa_rest(i):
                s = i % 2
                for g_ in range(16):
                    P.op("dve", lambda g, o=Vt[s][:, g_, 0:8], a=Ssb[s][:, g_, :]: g.max(out=o, in_=a), r=[BS[s]], w=[BV[s]])
                    P.op("dve", lambda g, o=Stmp[:], v=Vt[s][:, g_, 0:8], a=Ssb[s][:, g_, :]: g.match_replace(out=o, in_to_replace=v, in_values=a, imm_value=NEG),
                         r=[BS[s], BV[s]], w=[bf("Stmp")])
                    P.op("dve", lambda g, o=Vt[s][:, g_, 8:16], a=Stmp[:]: g.max(out=o, in_=a), r=[bf("Stmp")], w=[BV[s]])
                P.tt("dve", cand[:].rearrange("p h (a b) -> p h a b", a=16), Vt[s][:, 0:8, :].unsqueeze(3).to_broadcast([128, 8, 16, 16]),
                     Vt[s][:, 8:16, :].unsqueeze(2).to_broadcast([128, 8, 16, 16]), ALU.add, r=[BV[s]], w=[bf("cand")])
                for h in range(8):
                    P.op("dve", lambda g, o=SC[s][:, h, 0:8], a=cand[:, h, :]: g.max(out=o, in_=a), r=[bf("cand")], w=[BSC[s]])
                    P.op("dve", lambda g, o=candt[:], v=SC[s][:, h, 0:8], a=cand[:, h, :]: g.match_replace(out=o, in_to_replace=v, in_values=a, imm_value=NEG),
                         r=[bf("cand"), BSC[s]], w=[bf("candt")])
                    P.op("dve", lambda g, o=SC[s][:, h, 8:16], a=candt[:]: g.max(out=o, in_=a), r=[bf("candt")], w=[BSC[s]])

            def xa_stats(i):
                s = i % 2
                P.ts("dve", negm[s][:], SC[s][:, :, 0], -1.0, None, ALU.mult, r=[BSC[s]], w=[Bst[s]])
                P.tt("dve", ed[:], SC[s][:], negm[s][:].unsqueeze(2).to_broadcast([128, 8, 16]), ALU.add, r=[BSC[s], Bst[s]], w=[bf("ed")])
                P.act(ed[:], ed[:], AF.Exp, r=[bf("ed")], w=[bf("ed")])
                P.op("dve", lambda g, o=Zs[s][:], a=ed[:]: g.tensor_reduce(out=o, in_=a, axis=AX.X, op=ALU.add), r=[bf("ed")], w=[Bst[s]])
                P.act(nbias[s][:], Zs[s][:], AF.Ln, r=[Bst[s]], w=[Bst[s]])
                P.tt("dve", nbias[s][:], negm[s][:], nbias[s][:], ALU.subtract, r=[Bst[s]], w=[Bst[s]])
                P.act(coefE[s][:], nbias[s][:], AF.Exp, r=[Bst[s]], w=[Bst[s]])

            def xb(i, hh):
                s = i % 2
                A1, A2 = A1d[s][0], A2d[s][0]
                hs = slice(4 * hh, 4 * hh + 4)
                h2 = slice(8 + 4 * hh, 8 + 4 * hh + 4)
                P.tt("pool", sig[:, :, 0:4, :], Ssb[s][:, h2, :].unsqueeze(2).to_broadcast([128, 4, 4, 128]),
                     Vt[s][:, hs, 0:4].unsqueeze(3).to_broadcast([128, 4, 4, 128]), ALU.add, r=[BS[s], BV[s]], w=[bf("sig")])
                P.tt("dve", m1[:], Ssb[s][:, hs, :], Vt[s][:, hs, 3:4].to_broadcast([128, 4, 128]), ALU.is_ge, r=[BS[s], BV[s]], w=[bf("m1")])
                P.stt(S1m[:], m1[:], NEG, Ssb[s][:, hs, :], ALU.mult, ALU.add, r=[BS[s]], w=[bf("m1")])
                P.tt("pool", sig[:, :, 4:7, :], S1m[:].unsqueeze(2).to_broadcast([128, 4, 3, 128]),
                     Vt[s][:, h2, 0:3].unsqueeze(3).to_broadcast([128, 4, 3, 128]), ALU.add, r=[bf("m1"), BV[s]], w=[bf("sig")])
                P.act(Es[:], sig[:], AF.Exp, r=[bf("sig")], w=[bf("Es")])
                P.tt("dve", A1[:, hs, 0:4, :], Ssb[s][:, hs, :].unsqueeze(2).to_broadcast([128, 4, 4, 128]),
                     Vt[s][:, hs, 0:4].unsqueeze(3).to_broadcast([128, 4, 4, 128]), ALU.is_equal, r=[BS[s], BV[s]], w=[BA1[s]])
                P.tt("pool", A1[:, hs, 0:4, :], A1[:, hs, 0:4, :], coefE[s][:, hs].unsqueeze(2).unsqueeze(3).to_broadcast([128, 4, 4, 128]),
                     ALU.mult, r=[Bst[s]], w=[BA1[s]])
                P.tt("dve", A2[:, hs, 4:7, :], Ssb[s][:, h2, :].unsqueeze(2).to_broadcast([128, 4, 3, 128]),
                     Vt[s][:, h2, 0:3].unsqueeze(3).to_broadcast([128, 4, 3, 128]), ALU.is_equal, r=[BS[s], BV[s]], w=[BA2[s]])
                P.tt("pool", A2[:, hs, 4:7, :], A2[:, hs, 4:7, :], coefE[s][:, hs].unsqueeze(2).unsqueeze(3).to_broadcast([128, 4, 3, 128]),
                     ALU.mult, r=[Bst[s]], w=[BA2[s]])
                for hl in range(4):
                    h = 4 * hh + hl
                    P.stt(A2[:, h, 0:4, :], sig[:, hl, 0:4, :], SC[s][:, h, 15:16], Es[:, hl, 0:4, :], ALU.is_ge, ALU.mult,
                          r=[bf("sig"), bf("Es"), BSC[s]], w=[BA2[s]])
                    P.stt(A1[:, h, 4:7, :], sig[:, hl, 4:7, :], SC[s][:, h, 15:16], Es[:, hl, 4:7, :], ALU.is_ge, ALU.mult,
                          r=[bf("sig"), bf("Es"), BSC[s]], w=[BA1[s]])

            def y_tr(i, hf):
                s = i % 2
                p0 = hf * 64
                for side, (Asrc, nm, bA) in enumerate(((A1d[s][1], "A1", BA1[s]), (A2d[s][1], "A2", BA2[s]))):
                    for e8 in range(8):
                        pb = 4 + (e8 % 2) + 2 * side
                        for ee in range(8):
                            ep = e8 * 8 + ee
                            P.tr(ps[pb][0:NJ, ee * 64:(ee + 1) * 64], Asrc[p0:p0 + 64, :, :, ep].rearrange("p h a -> p (h a)"),
                                 identf[p0:p0 + 64, p0:p0 + 64], r=[bA, bf("identf")], w=[Bps[pb]])
                        pvb = ps[pb][:].bitcast(BF16)
                        if side == 0:
                            P.cp("act", A1T[0:NJ, e8 * 8:(e8 + 1) * 8, :, :].rearrange("p e t r -> p (e t r)"), pvb[0:NJ, :],
                                 r=[Bps[pb]], w=[bf("A1T")])
                        else:
                            P.cp("act", A2Tq[0:NJ, e8 * 8:(e8 + 1) * 8, :, :], pvb[0:NJ, :].rearrange("p (e t r) -> p e r t", e=8, r=2),
                                 r=[Bps[pb]], w=[bf("A2T")])

            def y_w(i, hf):
                WTs = WTs2[hf]
                bW = BWT[hf]
                for t4 in range(16):
                    pb = t4 % 4
                    for tt_ in range(4):
                        t = t4 * 4 + tt_
                        P.mm(ps[pb][:, tt_ * 128:(tt_ + 1) * 128], A2T[0:NJ, :, t], A1T[0:NJ, :, t, :], True, True,
                             r=[bf("A1T"), bf("A2T")], w=[Bps[pb]])
                    P.cp("act", WTs[:, :, t4 * 4:(t4 + 1) * 4], ps[pb][:].rearrange("p (t e) -> p e t", t=4),
                         r=[Bps[pb]], w=[bW])
                P.dma("sp", W_d[i * 2 + hf], WTs[:], r=[bW], w=[bf("W_d")])
                if debug and i == 0 and hf == 0:
                    P.dma("sp", dbg["W"], WTs[:].rearrange("p e t -> p (e t)"), r=[bW], w=[bf("dbg_W")])

            xa_s(0)
            xa_rest(0)
            xa_stats(0)
            xb(0, 0)
            xb(0, 1)
            for i in range(NTS):
                nxt = i + 1 < NTS
                if nxt:
                    xa_s(i + 1)
                y_tr(i, 0)
                if nxt:
                    xa_rest(i + 1)
                y_w(i, 0)
                y_tr(i, 1)
                if nxt:
                    xa_stats(i + 1)
                    xb(i + 1, 0)
                y_w(i, 1)
                if nxt:
                    xb(i + 1, 1)

        if PHASES >= 9:
            P.barrier()
            A.release(PM0)
            GELU = getattr(AF, os.environ.get("MK_GELU", "Gelu_apprx_tanh"))
            hn2Tb = sb("hn2Tb", [128, 8, 1024], BF16)
            junk = sb("junk", [128, D], BF16)
            acc = sb("acc", [128, 8, 1024])
            Ub = [sb("Ub%d" % i, [128, 8, 128], BF16) for i in range(3)]
            Vb = [sb("Vb%d" % i, [128, 8, 1024], BF16) for i in range(2)]
            Wblk = [sb("Wblk%d" % i, [128, 16, 8, 64], BF16) for i in range(2)]
            gel = [sb("gel%d" % i, [128, 512], BF16) for i in range(3)]
            Gg = [sb("Gg%d" % i, [128, 8, 1024], BF16) for i in range(2)]
            xt = [sb("xt%d" % i, [128, D]) for i in range(2)]
            x2 = [sb("x2%d" % i, [128, D]) for i in range(2)]
            ot = [sb("ot%d" % i, [128, D]) for i in range(2)]
            P.dma("sp", gB[:], gf.partition_broadcast(128), r=[bf("gB")], w=[bf("gB")])
            BU = [Buf() for _ in range(3)]
            BV = [Buf(), Buf()]
            BW = [Buf(), Buf()]
            Bgel = [Buf() for _ in range(3)]
            BG = [Buf(), Buf()]
            Bx2 = [Buf(), Buf()]
            Bot = [Buf(), Buf()]
            Bacc = [Buf() for _ in range(16)]
            def load_hn2Tb(Bk):
                for ti in range(8):
                    P.dma("sp", hn2Tb[:, :, ti * 128:(ti + 1) * 128], hn2T_d[Bk * 8 + ti].rearrange("p (k t) -> p k t", k=8),
                          r=[bf("hn2T_d")], w=[bf("hn2Tb")])

            def final_tile(Bk, ti):
                i = Bk * 8 + ti
                s = i % 2
                P.dma("sp", xt[s][:], x1_d[i * 128:(i + 1) * 128, :], r=[bf("x1_d")], w=[Bxt[s]])
                if debug:
                    P.dma("sp", dbg["pe"][i * 128:(i + 1) * 128, :], acc[:, ti, :], r=[Bacc[ti * 2], Bacc[ti * 2 + 1]], w=[bf("dbg_pe")])
                P.tt("dve", x2[s][:], xt[s][:], acc[:, ti, :], ALU.add, r=[Bxt[s], Bacc[ti * 2], Bacc[ti * 2 + 1]], w=[Bx2[s]])
                P.act(junk[:], x2[s][:], AF.Square, r=[Bx2[s]], w=[bf("junk"), Bss[s]], accum_out=ss[s][:, 0:1])
                P.act(rs[s][:, 0:1], ss[s][:, 0:1], AF.Sqrt, r=[Bss[s]], w=[Bss[s]], scale=1.0 / D, bias=EPS)
                P.op("dve", lambda g, a=rs[s][:, 0:1]: g.reciprocal(out=a, in_=a), r=[Bss[s]], w=[Bss[s]])
                P.stt(ot[s][:], x2[s][:], rs[s][:, 0:1], gB[:], ALU.mult, ALU.mult, r=[Bx2[s], Bss[s], bf("gB")], w=[Bot[s]])
                P.dma("sp", out[i * 128:(i + 1) * 128, :], ot[s][:], r=[Bot[s]], w=[bf("out")])

            def load_group(Bk, gI):
                s = gI % 2
                P.dma("sp", Wblk[s][:], W_d[Bk * 16:(Bk + 1) * 16, :, gI * 8:(gI + 1) * 8, :].rearrange("h p c t -> p h c t"),
                      r=[bf("W_d")], w=[BW[s]])

            load_hn2Tb(0)
            load_group(0, 0)
            for Bk in range(4):
                for gI in range(16):
                    s = gI % 2
                    nb_, ng_ = (Bk, gI + 1) if gI < 15 else (Bk + 1, 0)
                    if nb_ < 4:
                        load_group(nb_, ng_)
                    for cc in range(8):
                        c = gI * 8 + cc
                        u = c % 3
                        P.dma("pool", Ub[u][:].rearrange("p k e -> p (k e)"), UT[c], w=[BU[u]])
                        P.dma("pool", Vb[s][:, cc, :], Vd[c * 128:(c + 1) * 128, :], w=[BV[s]])
                        for nb in range(2):
                            pa = (c % 2) * 2 + nb
                            for kc in range(8):
                                P.mm(ps[pa][:], Ub[u][:, kc, :], hn2Tb[:, kc, nb * 512:(nb + 1) * 512], kc == 0, kc == 7,
                                     r=[BU[u], bf("hn2Tb")], w=[Bps[pa]])
                            gi = (c * 2 + nb) % 3
                            P.act(gel[gi][:], ps[pa][:], GELU, r=[Bps[pa]], w=[Bgel[gi]])
                            P.tt("dve", Gg[s][:, cc, nb * 512:(nb + 1) * 512].rearrange("p (h t) -> p h t", h=8),
                                 gel[gi][:].rearrange("p (h t) -> p h t", h=8), Wblk[s][:, nb * 8:(nb + 1) * 8, cc, :], ALU.mult,
                                 r=[Bgel[gi], BW[s]], w=[BG[s]])
                    if gI == 15 and Bk < 3:
                        load_hn2Tb(Bk + 1)
                    for ti in range(8):
                        for half in range(2):
                            po = 4 + (ti * 2 + half) % 4
                            for cc in range(8):
                                P.mm(ps[po][:], Gg[s][:, cc, ti * 128:(ti + 1) * 128], Vb[s][:, cc, half * 512:(half + 1) * 512], cc == 0, cc == 7,
                                     r=[BG[s], BV[s]], w=[Bps[po]])
                            dst = acc[:, ti, half * 512:(half + 1) * 512]
                            ba = Bacc[ti * 2 + half]
                            if gI == 0:
                                P.cp("dve", dst, ps[po][:], r=[Bps[po]], w=[ba])
                            else:
                                P.tt("dve", dst, ps[po][:], dst, ALU.add, r=[Bps[po], ba], w=[ba])
                        if gI == 15:
                            final_tile(Bk, ti)

        finals = [bf("out"), bf("dbg_pe"), bf("W_d"), bf("dbg_W"), bf("x1_d"), bf("hn2T_d"), bf("mixA_d"), bf("sgB_d"), bf("dbg_y"), bf("dbg_ob"), bf("dbg_c")]
        P.emit(finals)
    return nc


def make_in_maps(inputs):
    f32 = lambda a: np.ascontiguousarray(np.asarray(a, dtype=np.float32))
    convw_l = f32(np.asarray(inputs["conv_w"]).reshape(31, 8, 128).transpose(2, 1, 0))
    cvec = np.stack([np.asarray(inputs[k]).reshape(8, 128).T for k in ("conv_b", "conv_ln_g", "conv_ln_b")], axis=1)
    shared = {
        "w_in": f32(inputs["w_in"]), "norm1_g": f32(inputs["norm1_g"]), "norm2_g": f32(inputs["norm2_g"]),
        "normf_g": f32(inputs["normf_g"]), "convw_l": convw_l, "cvec_l": f32(cvec), "fox_bf": f32(inputs["fox_bf"]),
        "w_conv_out": f32(inputs["w_conv_out"]), "w_fox_out": f32(inputs["w_fox_out"]), "w_out": f32(inputs["w_out"]),
    }
    wq = np.asarray(inputs["peer_wq"], dtype=np.float32)
    wqT = wq.T.reshape(8, 2, 128, 1024).transpose(1, 0, 2, 3).reshape(16, 128, 1024)
    kk = np.stack([np.asarray(inputs["peer_k1"]), np.asarray(inputs["peer_k2"])], axis=0)
    kT = kk.transpose(0, 1, 3, 2).reshape(16, 128, 128)
    U = np.asarray(inputs["peer_u"], dtype=np.float32)
    UT = U.reshape(128, 128, 8, 128).transpose(0, 3, 2, 1).reshape(128, 128, 1024)
    shared.update({"wqT_l": f32(wqT), "kT_l": f32(kT), "UT_l": f32(UT), "peer_v": f32(inputs["peer_v"])})
    x = np.asarray(inputs["x"], dtype=np.float32)
    return [dict(shared, x=np.ascontiguousarray(x[b])) for b in range(x.shape[0])]


def kernel(**inputs):
    nc = build()
    in_maps = make_in_maps(inputs)
    res = run_bass_kernel_spmd(nc, in_maps, core_ids=list(range(8)))
    return np.stack([r["out"] for r in res.results], axis=0)
```
